# Optimizing a Trainium2 kernel written in Bass

```python
import math
import jax, jax.numpy as jnp
from jax import lax
import numpy as np

D_MODEL = 1024
BATCH = 8
SEQ = 2048
DEPTH = 1

D_MIX = D_MODEL
ATT_WIDTH = D_MIX // 2
POOL_WIDTH = D_MIX - ATT_WIDTH
HEAD_DIM = 64
N_HEADS = ATT_WIDTH // HEAD_DIM
IDX_HEADS = 4
IDX_DIM = 64
IDX_ROPE_DIM = 32
INDEX_TOPK = 256
POOL_WINDOWS = (2, 4, 8, 16)
N_POOL_GROUPS = len(POOL_WINDOWS)
POOL_CH = POOL_WIDTH // N_POOL_GROUPS
D_FF = 2816
ROPE_THETA = 10000.0
NORM_EPS = 1e-6
Q_BLOCK = 128
IN_SPLITS = (ATT_WIDTH, ATT_WIDTH, ATT_WIDTH, IDX_HEADS * IDX_DIM, IDX_DIM, IDX_HEADS, POOL_WIDTH)
IN_COLS = sum(IN_SPLITS)

kernel_name = "hybrid_dsa_multiscale_pool_macaron"


def rms_norm(x, g):
    xf = x.astype(jnp.float32)
    y = xf * lax.rsqrt(jnp.mean(xf * xf, axis=-1, keepdims=True) + NORM_EPS)
    return (y * g.astype(jnp.float32)).astype(x.dtype)


def rope(x, pos, rot_dim):
    half = rot_dim // 2
    inv = ROPE_THETA ** (-jnp.arange(half, dtype=jnp.float32) / half)
    ang = pos.astype(jnp.float32)[:, None] * inv[None, :]
    cos = jnp.cos(ang)[:, None, :]
    sin = jnp.sin(ang)[:, None, :]
    xr = x[..., :rot_dim].astype(jnp.float32)
    x1, x2 = xr[..., :half], xr[..., half:]
    rot = jnp.concatenate([x1 * cos - x2 * sin, x2 * cos + x1 * sin], axis=-1).astype(x.dtype)
    return jnp.concatenate([rot, x[..., rot_dim:]], axis=-1)


def swiglu(x, w_gate, w_up, w_down):
    return (jax.nn.silu(x @ w_gate) * (x @ w_up)) @ w_down


def dsa_attention(q, k, v, q_idx, k_idx, w_idx, top_k):
    B, S, H, Dh = q.shape
    nb = S // Q_BLOCK
    key_pos = jnp.arange(S)
    scale = Dh ** -0.5

    def to_blocks(a):
        return a.reshape((B, nb, Q_BLOCK) + a.shape[2:]).swapaxes(0, 1)

    def block(args):
        qb, qib, wb, posb = args
        rel = jax.nn.relu(jnp.einsum('bthd,bsd->bths', qib, k_idx).astype(jnp.float32))
        score = jnp.einsum('bth,bths->bts', wb.astype(jnp.float32), rel)
        causal = key_pos[None, :] <= posb[:, None]
        score = jnp.where(causal[None], score, -jnp.inf)
        _, idx = lax.top_k(score, top_k)
        valid = idx <= posb[None, :, None]
        k_sel = jax.vmap(lambda kk, ii: kk[ii])(k, idx)
        v_sel = jax.vmap(lambda vv, ii: vv[ii])(v, idx)
        logits = jnp.einsum('bthd,btkhd->bhtk', qb, k_sel).astype(jnp.float32) * scale
        logits = jnp.where(valid[:, None], logits, -jnp.inf)
        p = jax.nn.softmax(logits, axis=-1).astype(v.dtype)
        return jnp.einsum('bhtk,btkhd->bthd', p, v_sel)

    pos_blocks = jnp.arange(S).reshape(nb, Q_BLOCK)
    out = lax.map(block, (to_blocks(q), to_blocks(q_idx), to_blocks(w_idx), pos_blocks))
    return out.swapaxes(0, 1).reshape(B, S, H * Dh)


def multiscale_pool(u):
    B, S, _ = u.shape
    uf = u.astype(jnp.float32).reshape(B, S, N_POOL_GROUPS, POOL_CH)
    c = jnp.concatenate([jnp.zeros((B, 1, N_POOL_GROUPS, POOL_CH), jnp.float32),
                         jnp.cumsum(uf, axis=1)], axis=1)
    t = jnp.arange(S)
    outs = []
    for g, w in enumerate(POOL_WINDOWS):
        cg = c[:, :, g]
        c_lo = jnp.concatenate([jnp.zeros((B, w - 1, POOL_CH), jnp.float32), cg[:, :S + 1 - w]], axis=1)
        count = jnp.minimum(t + 1, w).astype(jnp.float32)[None, :, None]
        outs.append((cg[:, 1:] - c_lo) / count - uf[:, :, g])
    return jnp.stack(outs, axis=2).astype(u.dtype)


def setup_inputs(seed: int = 0) -> dict:
    key = jax.random.key(seed)
    ks = jax.random.split(key, 20)
    f32 = jnp.float32

    def nrm(k, shape, fan_in):
        return jax.random.normal(k, shape, f32) * fan_in ** -0.5

    def gain(k, shape):
        return 1.0 + 0.02 * jax.random.normal(k, shape, f32)

    L = DEPTH
    return {
        "x": jax.random.normal(ks[0], (BATCH, SEQ, D_MODEL), f32),
        "ffn1_norm": gain(ks[1], (L, D_MODEL)),
        "ffn1_w_gate": nrm(ks[2], (L, D_MODEL, D_FF), D_MODEL),
        "ffn1_w_up": nrm(ks[3], (L, D_MODEL, D_FF), D_MODEL),
        "ffn1_w_down": nrm(ks[4], (L, D_FF, D_MODEL), D_FF),
        "mix_norm": gain(ks[5], (L, D_MODEL)),
        "w_in": nrm(ks[6], (L, D_MODEL, IN_COLS), D_MODEL),
        "q_norm": gain(ks[7], (L, HEAD_DIM)),
        "k_norm": gain(ks[8], (L, HEAD_DIM)),
        "pool_w": nrm(ks[9], (L, N_POOL_GROUPS, POOL_CH, POOL_CH), POOL_CH),
        "pool_scale": 0.5 + 0.05 * jax.random.normal(ks[10], (L, POOL_WIDTH), f32),
        "w_out": nrm(ks[11], (L, D_MIX, D_MODEL), D_MIX),
        "ffn2_norm": gain(ks[12], (L, D_MODEL)),
        "ffn2_w_gate": nrm(ks[13], (L, D_MODEL, D_FF), D_MODEL),
        "ffn2_w_up": nrm(ks[14], (L, D_MODEL, D_FF), D_MODEL),
        "ffn2_w_down": nrm(ks[15], (L, D_FF, D_MODEL), D_FF),
    }


def reference(x, ffn1_norm, ffn1_w_gate, ffn1_w_up, ffn1_w_down, mix_norm, w_in,
              q_norm, k_norm, pool_w, pool_scale, w_out,
              ffn2_norm, ffn2_w_gate, ffn2_w_up, ffn2_w_down):
    B, S, _ = x.shape
    top_k = min(INDEX_TOPK, S // 4)
    pos = jnp.arange(S)
    split_points = list(np.cumsum(IN_SPLITS)[:-1])
    idx_scale = (IDX_HEADS ** -0.5) * (IDX_DIM ** -0.5)

    for l in range(DEPTH):
        x = x + 0.5 * swiglu(rms_norm(x, ffn1_norm[l]), ffn1_w_gate[l], ffn1_w_up[l], ffn1_w_down[l])

        h = rms_norm(x, mix_norm[l])
        proj = h @ w_in[l]
        q, k, v, qi, ki, wi, u = jnp.split(proj, split_points, axis=-1)
        q = rope(rms_norm(q.reshape(B, S, N_HEADS, HEAD_DIM), q_norm[l]), pos, HEAD_DIM)
        k = rope(rms_norm(k.reshape(B, S, N_HEADS, HEAD_DIM), k_norm[l]), pos, HEAD_DIM)
        v = v.reshape(B, S, N_HEADS, HEAD_DIM)
        qi = rope(qi.reshape(B, S, IDX_HEADS, IDX_DIM), pos, IDX_ROPE_DIM)
        ki = rope(ki.reshape(B, S, 1, IDX_DIM), pos, IDX_ROPE_DIM)[:, :, 0]
        wi = wi * idx_scale

        attn = dsa_attention(q, k, v, qi, ki, wi, top_k)

        pooled = jnp.einsum('bsgc,gcd->bsgd', multiscale_pool(u), pool_w[l])
        pooled = pooled.reshape(B, S, POOL_WIDTH) * pool_scale[l]

        x = x + jnp.concatenate([attn, pooled], axis=-1) @ w_out[l]

        x = x + 0.5 * swiglu(rms_norm(x, ffn2_norm[l]), ffn2_w_gate[l], ffn2_w_up[l], ffn2_w_down[l])
    return x
```

```python
import math
from contextlib import ExitStack
import numpy as np
import concourse.bass as bass
import concourse.mybir as mybir
from concourse.bass_utils import run_bass_kernel_spmd

F32 = mybir.dt.float32
BF16 = mybir.dt.bfloat16
AF = mybir.ActivationFunctionType
ALU = mybir.AluOpType
AX = mybir.AxisListType

SEQ = 2048
DM = 1024
DFF = 2816
NT = SEQ // 128
NCH = DFF // 128
EPS = 1e-6
TOPK = 256
NBIS = 16
NEG = -1.0e30
CQ, CK, CV, CQI, CKI, CWI, CU = 0, 512, 1024, 1536, 1792, 1856, 1860
O_CB, O_COSQ, O_SINQ, O_COSI, O_SINI, O_INVC = 0, 128, 640, 1152, 1408, 1664
O_G1, O_GM, O_G2, O_GQ, O_GK, O_PS = 1680, 1688, 1696, 1704, 1768, 1832
NCST = 1836


class _Op:
    __slots__ = ("eng", "fn", "deps", "idx", "is_dma", "sem", "val", "milestone")


class Sched:
    ENG = ("pe", "act", "dve", "pool", "sp")
    ROT = 900

    def __init__(self, nc, n_dma_sems=20):
        self.nc = nc
        self.ops = []
        self.last_w = {}
        self.readers = {}
        self.n_dma_sems = n_dma_sems
        self.cap = None

    def add(self, eng, fn, reads=(), writes=(), dma=False):
        if self.cap is not None:
            self.cap.append((eng, fn, tuple(reads), tuple(writes), dma))
            return None
        op = _Op()
        op.eng = eng; op.fn = fn; op.is_dma = dma; op.idx = len(self.ops)
        op.milestone = False; op.sem = None; op.val = 0
        deps = set()
        for r in reads:
            w = self.last_w.get(r)
            if w is not None:
                deps.add(w)
        for r in writes:
            w = self.last_w.get(r)
            if w is not None:
                deps.add(w)
            for rd in self.readers.get(r, ()):
                deps.add(rd)
        op.deps = sorted(deps)
        for r in reads:
            self.readers.setdefault(r, []).append(op.idx)
        for r in writes:
            self.last_w[r] = op.idx
            self.readers[r] = []
        self.ops.append(op)
        return op.idx

    def begin_capture(self):
        self.cap = []

    def end_capture(self):
        c = self.cap
        self.cap = None
        return c

    def merge(self, lists):
        lists = [l for l in lists if l]
        pos = [0] * len(lists)
        while True:
            best, bf = None, None
            for k, l in enumerate(lists):
                if pos[k] < len(l):
                    f = pos[k] / len(l)
                    if bf is None or f < bf:
                        best, bf = k, f
            if best is None:
                break
            self.add(*lists[best][pos[best]])
            pos[best] += 1

    def retire(self, pred):
        out = set()
        for r in list(self.last_w.keys()):
            if pred(r):
                out.add(self.last_w.pop(r))
        for r in list(self.readers.keys()):
            if pred(r):
                out.update(self.readers.pop(r))
        return out

    def seed(self, name, opset):
        self.readers.setdefault(name, []).extend(sorted(opset))

    def emit(self, stack):
        nc = self.nc
        ops = self.ops
        for op in ops:
            for d in op.deps:
                p = ops[d]
                if p.eng == "pe" and op.eng == "pe" and not p.is_dma and not op.is_dma:
                    continue
                p.milestone = True
        n_ms = {e: 0 for e in self.ENG}
        for op in ops:
            if op.milestone and not op.is_dma:
                n_ms[op.eng] += 1
        esems = {e: [stack.enter_context(nc.semaphore("s_%s%d" % (e, i)))
                     for i in range(n_ms[e] // self.ROT + 1)] for e in self.ENG}
        dsems = [stack.enter_context(nc.semaphore("d%d" % i)) for i in range(self.n_dma_sems)]
        ecount = {e: 0 for e in self.ENG}
        dcount = [0] * self.n_dma_sems
        dlast = [None] * self.n_dma_sems
        dnext = {e: 0 for e in self.ENG}
        prev_on_dsem = {}
        for op in ops:
            if op.is_dma:
                half = self.n_dma_sems // 2
                base = 0 if op.eng == "pool" else half
                j = base + dnext[op.eng] % half
                dnext[op.eng] += 1
                if dlast[j] is not None:
                    prev_on_dsem[op.idx] = dlast[j]
                dcount[j] += 16
                op.sem = dsems[j]; op.val = dcount[j]; dlast[j] = op.idx
                op.milestone = True
            elif op.milestone:
                c = ecount[op.eng]
                ecount[op.eng] += 1
                op.sem = esems[op.eng][c // self.ROT]; op.val = c % self.ROT + 1
        per_eng = {e: [o for o in ops if o.eng == e] for e in self.ENG}
        block = stack.enter_context(nc.Block())

        def run(engname, eng):
            waited = {}
            for op in per_eng[engname]:
                deps = list(op.deps)
                if op.idx in prev_on_dsem:
                    deps.append(prev_on_dsem[op.idx])
                need = {}
                for d in deps:
                    p = ops[d]
                    if p.eng == "pe" and op.eng == "pe" and not p.is_dma and not op.is_dma:
                        continue
                    key = id(p.sem)
                    if waited.get(key, 0) >= p.val:
                        continue
                    if key not in need or need[key][1] < p.val:
                        need[key] = (p.sem, p.val)
                for key, (sem, val) in need.items():
                    eng.wait_ge(sem, val)
                    waited[key] = val
                ins = op.fn(eng)
                if op.is_dma:
                    ins.then_inc(op.sem, 16)
                elif op.milestone:
                    ins.then_inc(op.sem, 1)

        @block.tensor
        def _(e):
            run("pe", e)

        @block.scalar
        def _(e):
            run("act", e)

        @block.vector
        def _(e):
            run("dve", e)

        @block.gpsimd
        def _(e):
            run("pool", e)

        @block.sync
        def _(e):
            run("sp", e)


def build_program():
    nc = bass.Bass("TRN2", target_bir_lowering=False)

    def din(name, shape):
        return nc.dram_tensor(name, shape, F32, kind="ExternalInput").ap()

    x_d = din("x", [SEQ, DM])
    wg_d = [din("ffn1_w_gate", [DM, DFF]), din("ffn2_w_gate", [DM, DFF])]
    wu_d = [din("ffn1_w_up", [DM, DFF]), din("ffn2_w_up", [DM, DFF])]
    wd_d = [din("ffn1_w_down", [DFF, DM]), din("ffn2_w_down", [DFF, DM])]
    win_d = din("w_in", [DM, 2372])
    wout_d = din("w_out", [DM, DM])
    poolw_d = din("pool_w", [4, 128, 128])
    cst_d = din("cst", [128, NCST])
    ident_d = din("ident", [128, 256])
    grep_d = din("grep", [3, 128, DM])
    out_d = nc.dram_tensor("out", [SEQ, DM], F32, kind="ExternalOutput").ap()

    with ExitStack() as st:
        def sb(name, shape, dt=F32):
            return st.enter_context(nc.sbuf_tensor(name, shape, dt))

        resid = sb("resid", [128, NT, DM], F32)
        RA = sb("RA", [128, 16384], BF16)
        RC = sb("RC", [128, 31232], BF16)
        RD = sb("RD", [128, 20480], BF16)
        cst = sb("cst_sb", [128, NCST], F32)
        identb = sb("identb", [128, 256], BF16)
        ones = sb("ones", [128, 64], F32)
        ss = sb("ss", [128, NT], F32)
        rstd = sb("rstd", [128, NT], F32)
        ss8 = sb("ss8", [128, 2, 8], F32)
        rs8 = sb("rs8", [128, 2, 8], F32)
        wraw = sb("wraw", [128, NT, 4], F32)
        wabs = sb("wabs", [128, NT, 4], F32)
        wsgn = sb("wsgn", [128, NT, 4], F32)
        bis = sb("bis", [128, 24], F32)
        PS = [st.enter_context(nc.psum_tensor("ps%d" % i, [128, 512], F32)) for i in range(8)]

        def view(reg, off_b, nbytes, dt):
            v = reg[:, off_b // 2:(off_b + nbytes) // 2]
            return v if dt == BF16 else v.bitcast(dt)

        KB = 1024
        S = Sched(nc)

        S.add("sp", lambda e: e.dma_start(out=cst[:], in_=cst_d[:]), writes=["cst"], dma=True)
        S.add("pool", lambda e: e.dma_start(out=identb[:], in_=ident_d[:]), writes=["identb"], dma=True)
        S.add("dve", lambda e: e.memset(ones[:], 1.0), writes=["ones"])
        S.add("dve", lambda e: e.memset(bis[:, 23:24], -30000.0), writes=["negbig"])
        cbias = cst[:, O_CB:O_CB + 128]

        def gT(off):
            return cst[:, off:off + 8]

        def psT_bf(k):
            return PS[k][:].bitcast(BF16)

        xn_bf = [view(RD, 16 * KB + i * 2 * KB, 2 * KB, BF16) for i in range(2)]
        xnT = RA[:].rearrange("p (k t) -> p k t", k=8)
        g_rep = view(RD, 24 * KB, 4 * KB, F32)

        def rms_blocks(gidx, load_x):
            S.add("sp", lambda e: e.dma_start(out=g_rep[:], in_=grep_d[gidx]), writes=["grep"], dma=True)
            stats, norms = [], []
            for b in range(4):
                bs = slice(4 * b, 4 * b + 4)
                S.begin_capture()
                S.add("dve", lambda e, bs=bs: e.memset(ss[:, bs], 0.0), writes=[("ss", b)])
                for i in range(4 * b, 4 * b + 4):
                    if load_x:
                        S.add("sp", lambda e, i=i: e.dma_start(out=resid[:, i, :], in_=x_d[i * 128:(i + 1) * 128, :]),
                              writes=[("resid", i, 0), ("resid", i, 1)], dma=True)
                    S.add("act", lambda e, i=i: e.activation(out=xn_bf[i % 2][:], in_=resid[:, i, :], func=AF.Square,
                                                             accum_out=ss[:, i:i + 1]),
                          reads=[("resid", i, 0), ("resid", i, 1), ("ss", b)], writes=[("xn_bf", i % 2), ("ssv", i)])
                S.add("act", lambda e, bs=bs: e.activation(out=rstd[:, bs], in_=ss[:, bs], func=AF.Sqrt, scale=1.0 / DM, bias=EPS),
                      reads=[("ssv", i) for i in range(4 * b, 4 * b + 4)] + [("ss", b)], writes=[("rstd0", b)])
                S.add("dve", lambda e, bs=bs: e.reciprocal(out=rstd[:, bs], in_=rstd[:, bs]),
                      reads=[("rstd0", b)], writes=[("rstd", b)])
                stats.append(S.end_capture())
                S.begin_capture()
                for i in range(4 * b, 4 * b + 4):
                    S.add("dve", lambda e, i=i: e.scalar_tensor_tensor(
                        out=xn_bf[i % 2][:], in0=resid[:, i, :], scalar=rstd[:, i:i + 1], in1=g_rep[:],
                        op0=ALU.mult, op1=ALU.mult),
                        reads=[("resid", i, 0), ("resid", i, 1), ("rstd", b), "grep"], writes=[("xn_bf", i % 2)])
                    pk = 6 + (i % 2)
                    for k in range(8):
                        S.add("pe", lambda e, i=i, k=k, pk=pk: e.transpose(
                            out=psT_bf(pk)[:, k * 128:(k + 1) * 128], in_=xn_bf[i % 2][:, k * 128:(k + 1) * 128],
                            identity=identb[:, 0:128]),
                            reads=[("xn_bf", i % 2), "identb"], writes=[("ps", pk)])
                    S.add("act", lambda e, i=i, pk=pk: e.activation(
                        out=xnT[:, :, i * 128:(i + 1) * 128],
                        in_=psT_bf(pk).rearrange("p (k t) -> p k t", k=8), func=AF.Copy),
                        reads=[("ps", pk)], writes=[("xnT", i)])
                norms.append(S.end_capture())
            return stats, norms

        def emit_norm_interleaved(sn, body, n_head, unit):
            stats, norms = sn
            seq = [body[:n_head], stats[0], stats[1], norms[0], body[n_head:n_head + unit],
                   stats[2], norms[1], body[n_head + unit:n_head + 2 * unit],
                   stats[3], norms[2], body[n_head + 2 * unit:n_head + 3 * unit],
                   norms[3], body[n_head + 3 * unit:]]
            for l in seq:
                for o in l:
                    S.add(*o)

        GROUPS = [(0, 6), (6, 6), (12, 6), (18, 4)]
        hT = view(RC, 0, 24 * KB, BF16).rearrange("p (c t) -> p c t", c=6)
        wdb = [view(RC, 24 * KB + i * 12 * KB, 12 * KB, BF16).rearrange("p (c d) -> p c d", c=6) for i in range(2)]
        wgb = [view(RD, i * 4 * KB, 4 * KB, BF16).rearrange("p (k f) -> p k f", k=8) for i in range(2)]
        wub = [view(RD, 8 * KB + i * 4 * KB, 4 * KB, BF16).rearrange("p (k f) -> p k f", k=8) for i in range(2)]
        sgb = [view(RD, 20 * KB + i * 2 * KB, 2 * KB, F32) for i in range(2)]

        def ffn(li, goff, load_x, final):
            sn = rms_blocks(goff, load_x)
            S.begin_capture()
            pair_ctr = [0]
            for gi, (c0, ncg) in enumerate(GROUPS):
                wd_t = wdb[gi % 2]

                def _wd_dma(c0=c0, ncg=ncg, wd_t=wd_t, gi=gi):
                    S.add("pool", lambda e: e.dma_start(
                        out=wd_t[:, 0:ncg, :],
                        in_=wd_d[li][c0 * 128:(c0 + ncg) * 128, :].rearrange("(c p) d -> p c d", p=128)),
                        writes=[("wdb", gi % 2)], dma=True)
                for pr in range(ncg // 2):
                    j = pair_ctr[0] % 2
                    pair_ctr[0] += 1
                    col0 = (c0 + 2 * pr) * 128
                    S.add("pool", lambda e, j=j, col0=col0: e.dma_start(
                        out=wgb[j][:], in_=wg_d[li][:, col0:col0 + 256].rearrange("(k p) f -> p k f", p=128)),
                        writes=[("wgb", j)], dma=True)
                    S.add("pool", lambda e, j=j, col0=col0: e.dma_start(
                        out=wub[j][:], in_=wu_d[li][:, col0:col0 + 256].rearrange("(k p) f -> p k f", p=128)),
                        writes=[("wub", j)], dma=True)
                    if pr == 0:
                        _wd_dma()
                    for cc in range(2):
                        cl = 2 * pr + cc
                        for tb in range(4):
                            q = (cl * 4 + tb) % 2
                            pg, pu = q, 2 + q
                            for k in range(8):
                                S.add("pe", lambda e, j=j, cc=cc, tb=tb, k=k, pg=pg: e.matmul(
                                    PS[pg][:], lhsT=wgb[j][:, k, cc * 128:(cc + 1) * 128],
                                    rhs=xnT[:, k, tb * 512:(tb + 1) * 512], start=(k == 0), stop=(k == 7)),
                                    reads=[("wgb", j)] + [("xnT", 4 * tb + t) for t in range(4)], writes=[("ps", pg)])
                            for k in range(8):
                                S.add("pe", lambda e, j=j, cc=cc, tb=tb, k=k, pu=pu: e.matmul(
                                    PS[pu][:], lhsT=wub[j][:, k, cc * 128:(cc + 1) * 128],
                                    rhs=xnT[:, k, tb * 512:(tb + 1) * 512], start=(k == 0), stop=(k == 7)),
                                    reads=[("wub", j)] + [("xnT", 4 * tb + t) for t in range(4)], writes=[("ps", pu)])
                            S.add("act", lambda e, q=q, pg=pg: e.activation(out=sgb[q][:], in_=PS[pg][:], func=AF.Silu),
                                  reads=[("ps", pg)], writes=[("sgb", q)])
                            S.add("dve", lambda e, q=q, pu=pu, cl=cl, tb=tb: e.tensor_tensor(
                                out=hT[:, cl, tb * 512:(tb + 1) * 512], in0=sgb[q][:], in1=PS[pu][:], op=ALU.mult),
                                reads=[("sgb", q), ("ps", pu)], writes=[("hT", cl, tb)])
                for i in range(NT):
                    for dh in range(2):
                        pd = 4 + (i * 2 + dh) % 2
                        for cl in range(ncg):
                            S.add("pe", lambda e, i=i, dh=dh, cl=cl, pd=pd, wd_t=wd_t, ncg=ncg: e.matmul(
                                PS[pd][:], lhsT=hT[:, cl, i * 128:(i + 1) * 128],
                                rhs=wd_t[:, cl, dh * 512:(dh + 1) * 512], start=(cl == 0), stop=(cl == ncg - 1)),
                                reads=[("hT", cl, i // 4), ("wdb", gi % 2)], writes=[("ps", pd)])
                        S.add("dve", lambda e, i=i, dh=dh, pd=pd: e.scalar_tensor_tensor(
                            out=resid[:, i, dh * 512:(dh + 1) * 512], in0=PS[pd][:], scalar=0.5,
                            in1=resid[:, i, dh * 512:(dh + 1) * 512], op0=ALU.mult, op1=ALU.add),
                            reads=[("ps", pd), ("resid", i, dh)], writes=[("resid", i, dh)])
                    if final and gi == len(GROUPS) - 1:
                        S.add("sp", lambda e, i=i: e.dma_start(out=out_d[i * 128:(i + 1) * 128, :], in_=resid[:, i, :]),
                              reads=[("resid", i, 0), ("resid", i, 1)], writes=[("out", i)], dma=True)
            body = S.end_capture()
            emit_norm_interleaved(sn, body, 3, 18)

        ffn(0, 0, True, False)

        mix_sn = rms_blocks(1, False)
        dead = S.retire(lambda r: isinstance(r, tuple) and r[0] in ("hT", "wdb", "wgb", "wub", "sgb"))
        for nm in ["ubr", "wst0", "wst1", "mixscr"]:
            S.seed(nm, dead)
        S.begin_capture()

        UW = 16 + SEQ
        ubuf = [view(RC, i * 8256, 8256, F32) for i in range(3)]
        p2T = view(RC, 25 * KB, 16 * KB, BF16).rearrange("p (g t) -> p g t", g=4)
        woutp = view(RC, 41 * KB, 8 * KB, BF16).rearrange("p (g d) -> p g d", g=4)
        plT = view(RC, 49 * KB, 4 * KB, BF16)
        poolw = view(RC, 53 * KB, 1 * KB, BF16).rearrange("p (g d) -> p g d", g=4)
        wst = [view(RD, i * 8 * KB, 8 * KB, BF16).rearrange("p (k f) -> p k f", k=8) for i in range(2)]
        S.add("pool", lambda e: e.dma_start(out=wst[0][:], in_=win_d[:, CU:CU + 512].rearrange("(k p) f -> p k f", p=128)),
              writes=["wst0"], dma=True)
        S.add("pool", lambda e: e.dma_start(out=woutp[:], in_=wout_d[512:1024, :].rearrange("(g p) d -> p g d", p=128)),
              writes=["ubr"], dma=True)
        S.add("pool", lambda e: e.dma_start(out=poolw[:], in_=poolw_d.rearrange("g c d -> c g d")),
              writes=["ubr"], dma=True)
        for b in range(3):
            S.add("dve", lambda e, b=b: e.memset(ubuf[b][:, 0:16], 0.0), writes=["ubr"])
        for g in range(4):
            u, sa, sbf = ubuf[0], ubuf[1], ubuf[2]
            for tb in range(4):
                pp = tb % 2
                for k in range(8):
                    S.add("pe", lambda e, g=g, tb=tb, k=k, pp=pp: e.matmul(
                        PS[pp][:], lhsT=wst[0][:, k, g * 128:(g + 1) * 128], rhs=xnT[:, k, tb * 512:(tb + 1) * 512],
                        start=(k == 0), stop=(k == 7)),
                        reads=["wst0"] + [("xnT", 4 * tb + t) for t in range(4)], writes=[("ps", pp)])
                S.add("act", lambda e, tb=tb, pp=pp, u=u: e.activation(out=u[:, 16 + tb * 512:16 + (tb + 1) * 512],
                                                                        in_=PS[pp][:], func=AF.Copy),
                      reads=[("ps", pp)], writes=["ubr"])
            cur = u
            nxt = [sa, sbf]
            for stp in range(g + 1):
                sh = 1 << stp
                dst = nxt[stp % 2]
                S.add("dve", lambda e, cur=cur, dst=dst, sh=sh: e.tensor_tensor(
                    out=dst[:, 16:UW], in0=cur[:, 16:UW], in1=cur[:, 16 - sh:UW - sh], op=ALU.add),
                    reads=["ubr"], writes=["ubr"])
                cur = dst
            w = 2 << g
            S.add("dve", lambda e, cur=cur, u=u, w=w: e.scalar_tensor_tensor(
                out=plT[:], in0=cur[:, 16:UW], scalar=1.0 / w, in1=u[:, 16:UW], op0=ALU.mult, op1=ALU.subtract),
                reads=["ubr"], writes=["ubr"])
            tmpc = nxt[(g + 1) % 2]
            S.add("dve", lambda e, cur=cur, tmpc=tmpc, w=w: e.tensor_tensor(
                out=tmpc[:, 16:16 + w - 1], in0=cur[:, 16:16 + w - 1], in1=cst[:, O_INVC:O_INVC + w - 1], op=ALU.mult),
                reads=["ubr", "cst"], writes=["ubr"])
            S.add("dve", lambda e, tmpc=tmpc, u=u, w=w: e.tensor_tensor(
                out=plT[:, 0:w - 1], in0=tmpc[:, 16:16 + w - 1], in1=u[:, 16:16 + w - 1], op=ALU.subtract),
                reads=["ubr"], writes=["ubr"])
            for tb in range(4):
                pp = 2 + tb % 2
                S.add("pe", lambda e, g=g, tb=tb, pp=pp: e.matmul(
                    PS[pp][:], lhsT=poolw[:, g, :], rhs=plT[:, tb * 512:(tb + 1) * 512], start=True, stop=True),
                    reads=["ubr"], writes=[("ps", pp)])
                S.add("act", lambda e, g=g, tb=tb, pp=pp: e.activation(
                    out=p2T[:, g, tb * 512:(tb + 1) * 512], in_=PS[pp][:], func=AF.Copy,
                    scale=cst[:, O_PS + g:O_PS + g + 1]),
                    reads=[("ps", pp), "cst"], writes=["ubr"])
        for i in range(NT):
            for dh in range(2):
                pd = 4 + (i * 2 + dh) % 2
                for g in range(4):
                    S.add("pe", lambda e, i=i, dh=dh, g=g, pd=pd: e.matmul(
                        PS[pd][:], lhsT=p2T[:, g, i * 128:(i + 1) * 128], rhs=woutp[:, g, dh * 512:(dh + 1) * 512],
                        start=(g == 0), stop=(g == 3)),
                        reads=["ubr"], writes=[("ps", pd)])
                S.add("dve", lambda e, i=i, dh=dh, pd=pd: e.tensor_tensor(
                    out=resid[:, i, dh * 512:(dh + 1) * 512], in0=PS[pd][:], in1=resid[:, i, dh * 512:(dh + 1) * 512],
                    op=ALU.add),
                    reads=[("ps", pd), ("resid", i, dh)], writes=[("resid", i, dh)])
        ubody = S.end_capture()
        emit_norm_interleaved(mix_sn, ubody, 6, 9)
        dead_g = S.retire(lambda r: r == "grep")
        for nm in ["qsq", "qn", "grep"]:
            S.seed(nm, dead_g)
        dead = S.retire(lambda r: r == "ubr")
        for nm in ["qT", "kT", "V", "qiT", "kiT2"]:
            S.seed(nm, dead)

        qT = view(RC, 0, 16 * KB, BF16).rearrange("p (h t) -> p h t", h=4)
        kT = view(RC, 16 * KB, 16 * KB, BF16).rearrange("p (h t) -> p h t", h=4)
        Vt = view(RC, 32 * KB, 16640, BF16).rearrange("p (i h d) -> p i h d", i=NT, h=8)
        qiT = view(RC, 32 * KB + 16640, 8 * KB, BF16).rearrange("p (h t) -> p h t", h=2)
        kiT2 = view(RC, 40 * KB + 16640, 4 * KB, BF16)
        MS = 20 * KB
        qraw = [view(RD, MS + i * 2 * KB, 2 * KB, F32) for i in range(2)]
        qsq = view(RD, MS + 4 * KB, 2 * KB, F32)
        qn = view(RD, MS + 6 * KB, 2 * KB, F32)
        rt = [view(RD, MS + 8 * KB + i * KB, KB, F32) for i in range(4)]
        qr = [view(RD, MS + 12 * KB + i * KB, KB, BF16) for i in range(2)]
        iraw = view(RD, MS + 14 * KB, 1296, F32)
        qib_l = [view(RD, MS + 14 * KB + 1296, 512, BF16), view(RD, MS + 14 * KB + 3088, 512, BF16)]
        ki2b_l = [view(RD, MS + 14 * KB + 1808, 256, BF16), view(RD, MS + 14 * KB + 3600, 256, BF16)]
        it = [view(RD, MS + 14 * KB + 2064 + i * 256, 256, F32) for i in range(4)]

        S.add("dve", lambda e: e.memset(Vt[:, :, :, 64:65], 1.0), writes=["V"])

        groups = [("q", CQ, 512), ("k", CK, 512), ("v", CV, 512), ("i", CQI, 324)]
        for gi, (gname, coff, ncol) in enumerate(groups):
            wj = (gi + 1) % 2
            S.add("pool", lambda e, wj=wj, coff=coff, ncol=ncol: e.dma_start(
                out=wst[wj][:, :, 0:ncol], in_=win_d[:, coff:coff + ncol].rearrange("(k p) f -> p k f", p=128)),
                writes=["wst%d" % wj], dma=True)
            tile_ops = []
            for i in range(NT):
                S.begin_capture()
                pp = i % 2
                for k in range(8):
                    S.add("pe", lambda e, i=i, k=k, pp=pp, wj=wj, ncol=ncol: e.matmul(
                        PS[pp][:, 0:ncol], lhsT=xnT[:, k, i * 128:(i + 1) * 128], rhs=wst[wj][:, k, 0:ncol],
                        start=(k == 0), stop=(k == 7)),
                        reads=["wst%d" % wj, ("xnT", i)], writes=[("ps", pp)])
                if gname in ("q", "k"):
                    goff = O_GQ if gname == "q" else O_GK
                    dstT = qT if gname == "q" else kT
                    qa = qraw[i % 2]
                    qrb = qr[i % 2]
                    q3 = qa.rearrange("p (h d) -> p h d", h=8)
                    S.add("act", lambda e, pp=pp, qa=qa: e.activation(out=qa[:], in_=PS[pp][:], func=AF.Copy),
                          reads=[("ps", pp)], writes=[("qraw", i % 2)])
                    S.add("dve", lambda e, qa=qa: e.tensor_tensor(out=qsq[:], in0=qa[:], in1=qa[:], op=ALU.mult),
                          reads=[("qraw", i % 2)], writes=["qsq"])
                    S.add("dve", lambda e, i=i: e.tensor_reduce(out=ss8[:, i % 2, :], in_=qsq.rearrange("p (h d) -> p h d", h=8),
                                                                axis=AX.X, op=ALU.add),
                          reads=["qsq"], writes=[("ss8", i % 2)])
                    S.add("act", lambda e, i=i: e.activation(out=rs8[:, i % 2, :], in_=ss8[:, i % 2, :], func=AF.Sqrt,
                                                             scale=1.0 / 64, bias=EPS),
                          reads=[("ss8", i % 2)], writes=[("rs8a", i % 2)])
                    qn3 = qn.rearrange("p (h d) -> p h d", h=8)
                    S.add("dve", lambda e, goff=goff, q3=q3: e.tensor_tensor(
                        out=qn3, in0=q3, in1=cst[:, goff:goff + 64].unsqueeze(1).to_broadcast([128, 8, 64]), op=ALU.mult),
                        reads=[("qraw", i % 2), "cst"], writes=["qn"])
                    S.add("dve", lambda e, i=i: e.reciprocal(out=rs8[:, i % 2, :], in_=rs8[:, i % 2, :]),
                          reads=[("rs8a", i % 2)], writes=[("rs8", i % 2)])
                    S.add("dve", lambda e, i=i: e.tensor_tensor(
                        out=qn3, in0=qn3, in1=rs8[:, i % 2, :].unsqueeze(2).to_broadcast([128, 8, 64]), op=ALU.mult),
                        reads=["qn", ("rs8", i % 2)], writes=["qn"])
                    cosb = cst[:, O_COSQ + i * 32:O_COSQ + (i + 1) * 32].unsqueeze(1).to_broadcast([128, 8, 32])
                    sinb = cst[:, O_SINQ + i * 32:O_SINQ + (i + 1) * 32].unsqueeze(1).to_broadcast([128, 8, 32])
                    r3 = [t.rearrange("p (h d) -> p h d", h=8) for t in rt]
                    qr3 = qrb.rearrange("p (h d) -> p h d", h=8)
                    x1, x2 = qn3[:, :, 0:32], qn3[:, :, 32:64]
                    S.add("dve", lambda e, x1=x1, cosb=cosb, r3=r3: e.tensor_tensor(out=r3[0], in0=x1, in1=cosb, op=ALU.mult),
                          reads=["qn", "cst"], writes=[("rt", 0)])
                    S.add("dve", lambda e, x2=x2, sinb=sinb, r3=r3: e.tensor_tensor(out=r3[1], in0=x2, in1=sinb, op=ALU.mult),
                          reads=["qn", "cst"], writes=[("rt", 1)])
                    S.add("dve", lambda e, r3=r3, qr3=qr3: e.tensor_tensor(out=qr3[:, :, 0:32], in0=r3[0], in1=r3[1], op=ALU.subtract),
                          reads=[("rt", 0), ("rt", 1)], writes=[("qr", i % 2)])
                    S.add("dve", lambda e, x2=x2, cosb=cosb, r3=r3: e.tensor_tensor(out=r3[2], in0=x2, in1=cosb, op=ALU.mult),
                          reads=["qn", "cst"], writes=[("rt", 2)])
                    S.add("dve", lambda e, x1=x1, sinb=sinb, r3=r3: e.tensor_tensor(out=r3[3], in0=x1, in1=sinb, op=ALU.mult),
                          reads=["qn", "cst"], writes=[("rt", 3)])
                    S.add("dve", lambda e, r3=r3, qr3=qr3: e.tensor_tensor(out=qr3[:, :, 32:64], in0=r3[2], in1=r3[3], op=ALU.add),
                          reads=[("rt", 2), ("rt", 3)], writes=[("qr", i % 2)])
                    pk = 6 + (i % 2)
                    for hp in range(4):
                        S.add("pe", lambda e, hp=hp, pk=pk, qrb=qrb: e.transpose(
                            out=psT_bf(pk)[:, hp * 128:(hp + 1) * 128], in_=qrb[:, hp * 128:(hp + 1) * 128],
                            identity=identb[:, 0:128]),
                            reads=[("qr", i % 2), "identb"], writes=[("ps", pk)])
                    S.add("act", lambda e, i=i, pk=pk, dstT=dstT: e.activation(
                        out=dstT[:, :, i * 128:(i + 1) * 128],
                        in_=psT_bf(pk)[:, 0:512].rearrange("p (h t) -> p h t", h=4), func=AF.Copy),
                        reads=[("ps", pk)], writes=[(gname + "T", i)])
                elif gname == "v":
                    S.add("act", lambda e, i=i, pp=pp: e.activation(
                        out=Vt[:, i, :, 0:64], in_=PS[pp][:].rearrange("p (h d) -> p h d", h=8), func=AF.Copy),
                        reads=[("ps", pp)], writes=[("Vt", i)])
                else:
                    S.add("act", lambda e, pp=pp: e.activation(out=iraw[:], in_=PS[pp][:, 0:324], func=AF.Copy),
                          reads=[("ps", pp)], writes=["iraw"])
                    qi3 = iraw[:, 0:256].rearrange("p (h d) -> p h d", h=4)
                    qib = qib_l[i % 2]
                    ki2b = ki2b_l[i % 2]
                    QIB = ("qib", i % 2)
                    KI2B = ("ki2b", i % 2)
                    qib3 = qib.rearrange("p (h d) -> p h d", h=4)
                    cosb = cst[:, O_COSI + i * 16:O_COSI + (i + 1) * 16].unsqueeze(1).to_broadcast([128, 4, 16])
                    sinb = cst[:, O_SINI + i * 16:O_SINI + (i + 1) * 16].unsqueeze(1).to_broadcast([128, 4, 16])
                    i3 = [t.rearrange("p (h d) -> p h d", h=4) for t in it]
                    x1, x2 = qi3[:, :, 0:16], qi3[:, :, 16:32]
                    S.add("dve", lambda e, x1=x1, cosb=cosb, i3=i3: e.tensor_tensor(out=i3[0], in0=x1, in1=cosb, op=ALU.mult),
                          reads=["iraw", "cst"], writes=[("it", 0)])
                    S.add("dve", lambda e, x2=x2, sinb=sinb, i3=i3: e.tensor_tensor(out=i3[1], in0=x2, in1=sinb, op=ALU.mult),
                          reads=["iraw", "cst"], writes=[("it", 1)])
                    S.add("dve", lambda e, i3=i3, qib3=qib3: e.tensor_tensor(out=qib3[:, :, 0:16], in0=i3[0], in1=i3[1], op=ALU.subtract),
                          reads=[("it", 0), ("it", 1)], writes=[QIB])
                    S.add("dve", lambda e, x2=x2, cosb=cosb, i3=i3: e.tensor_tensor(out=i3[2], in0=x2, in1=cosb, op=ALU.mult),
                          reads=["iraw", "cst"], writes=[("it", 2)])
                    S.add("dve", lambda e, x1=x1, sinb=sinb, i3=i3: e.tensor_tensor(out=i3[3], in0=x1, in1=sinb, op=ALU.mult),
                          reads=["iraw", "cst"], writes=[("it", 3)])
                    S.add("dve", lambda e, i3=i3, qib3=qib3: e.tensor_tensor(out=qib3[:, :, 16:32], in0=i3[2], in1=i3[3], op=ALU.add),
                          reads=[("it", 2), ("it", 3)], writes=[QIB])
                    S.add("dve", lambda e, qi3=qi3, qib3=qib3: e.tensor_copy(out=qib3[:, :, 32:64], in_=qi3[:, :, 32:64]),
                          reads=["iraw"], writes=[QIB])
                    kr = iraw[:, 256:320]
                    cos1 = cst[:, O_COSI + i * 16:O_COSI + (i + 1) * 16]
                    sin1 = cst[:, O_SINI + i * 16:O_SINI + (i + 1) * 16]
                    S.add("dve", lambda e, kr=kr, cos1=cos1: e.tensor_tensor(out=it[0][:, 0:16], in0=kr[:, 0:16], in1=cos1, op=ALU.mult),
                          reads=["iraw", "cst", QIB], writes=[("it", 0)])
                    S.add("dve", lambda e, kr=kr, sin1=sin1: e.tensor_tensor(out=it[1][:, 0:16], in0=kr[:, 16:32], in1=sin1, op=ALU.mult),
                          reads=["iraw", "cst", QIB], writes=[("it", 1)])
                    S.add("dve", lambda e, ki2b=ki2b: e.tensor_tensor(out=ki2b[:, 0:16], in0=it[0][:, 0:16], in1=it[1][:, 0:16], op=ALU.subtract),
                          reads=[("it", 0), ("it", 1)], writes=[KI2B])
                    S.add("dve", lambda e, kr=kr, cos1=cos1: e.tensor_tensor(out=it[2][:, 0:16], in0=kr[:, 16:32], in1=cos1, op=ALU.mult),
                          reads=["iraw", "cst", QIB], writes=[("it", 2)])
                    S.add("dve", lambda e, kr=kr, sin1=sin1: e.tensor_tensor(out=it[3][:, 0:16], in0=kr[:, 0:16], in1=sin1, op=ALU.mult),
                          reads=["iraw", "cst", QIB], writes=[("it", 3)])
                    S.add("dve", lambda e, ki2b=ki2b: e.tensor_tensor(out=ki2b[:, 16:32], in0=it[2][:, 0:16], in1=it[3][:, 0:16], op=ALU.add),
                          reads=[("it", 2), ("it", 3)], writes=[KI2B])
                    S.add("dve", lambda e, kr=kr, ki2b=ki2b: e.tensor_copy(out=ki2b[:, 32:64], in_=kr[:, 32:64]),
                          reads=["iraw"], writes=[KI2B])
                    S.add("dve", lambda e, ki2b=ki2b: e.tensor_copy(out=ki2b[:, 64:128], in_=ki2b[:, 0:64]),
                          reads=[KI2B], writes=[KI2B])
                    S.add("dve", lambda e, i=i: e.tensor_copy(out=wraw[:, i, :], in_=iraw[:, 320:324]),
                          reads=["iraw"], writes=[("wraw", i)])
                    pk = 6 + (i % 2)
                    for hp in range(2):
                        S.add("pe", lambda e, hp=hp, pk=pk, qib=qib: e.transpose(
                            out=psT_bf(pk)[:, hp * 128:(hp + 1) * 128], in_=qib[:, hp * 128:(hp + 1) * 128],
                            identity=identb[:, 0:128]),
                            reads=[QIB, "identb"], writes=[("ps", pk)])
                    S.add("pe", lambda e, pk=pk, ki2b=ki2b: e.transpose(
                        out=psT_bf(pk)[:, 256:384], in_=ki2b[:], identity=identb[:, 0:128]),
                        reads=[KI2B, "identb"], writes=[("ps", pk)])
                    S.add("act", lambda e, i=i, pk=pk: e.activation(
                        out=qiT[:, :, i * 128:(i + 1) * 128],
                        in_=psT_bf(pk)[:, 0:256].rearrange("p (h t) -> p h t", h=2), func=AF.Copy),
                        reads=[("ps", pk)], writes=[("qiT", i)])
                    S.add("act", lambda e, i=i, pk=pk: e.activation(
                        out=kiT2[:, i * 128:(i + 1) * 128], in_=psT_bf(pk)[:, 256:384], func=AF.Copy),
                        reads=[("ps", pk)], writes=[("kiT2", i)])
                tile_ops.append(S.end_capture())
            def _split(ops):
                for j, o in enumerate(ops):
                    if o[0] == 'pe' and any(w in (('ps', 6), ('ps', 7)) for w in o[3]):
                        return ops[:j], ops[j:]
                return ops, []
            parts = [_split(o) for o in tile_ops]
            for o in parts[0][0]:
                S.add(*o)
            for i in range(NT):
                if i + 1 < NT:
                    for o in parts[i + 1][0]:
                        S.add(*o)
                for o in parts[i][1]:
                    S.add(*o)
        wr2 = wraw[:].rearrange("p i h -> p (i h)")
        wa2 = wabs[:].rearrange("p i h -> p (i h)")
        ws2 = wsgn[:].rearrange("p i h -> p (i h)")
        allw = [("wraw", i) for i in range(NT)]
        S.add("dve", lambda e: e.tensor_scalar(out=wa2, in0=wr2, scalar1=-1.0, scalar2=None, op0=ALU.mult),
              reads=allw, writes=["wabs0"])
        S.add("dve", lambda e: e.tensor_tensor(out=wa2, in0=wa2, in1=wr2, op=ALU.max),
              reads=allw + ["wabs0"], writes=["wabs1"])
        S.add("dve", lambda e: e.tensor_scalar(out=wa2, in0=wa2, scalar1=1.0 / 16, scalar2=None, op0=ALU.mult),
              reads=["wabs1"], writes=["wabs"])
        S.add("dve", lambda e: e.tensor_scalar(out=ws2, in0=wr2, scalar1=0.0, scalar2=2.0, op0=ALU.is_ge, op1=ALU.mult),
              reads=allw, writes=["wsgn0"])
        S.add("dve", lambda e: e.tensor_scalar(out=ws2, in0=ws2, scalar1=-1.0, scalar2=None, op0=ALU.add),
              reads=["wsgn0"], writes=["wsgn"])

        dead = S.retire(lambda r: (isinstance(r, tuple) and r[0] in ("xnT", "xn_bf", "qraw", "rt", "qr", "it", "qib", "ki2b"))
                        or r in ("wst0", "wst1", "mixscr", "qsq", "qn", "iraw"))
        for nm in (["wouta", "attnT", "attn_tm"] + [("jm", a, b) for a in range(2) for b in range(2)]
                   + [("pT", i) for i in range(4)]
                   + [("acc", t, i) for t in range(2) for i in range(4)]):
            S.seed(nm, dead)
        QB = 256
        NQB = SEQ // QB
        accb = [view(RA, t * 8 * KB, 8 * KB, F32) for t in range(2)]
        junkD = view(RA, 16 * KB, 4 * KB, BF16)
        junkA = view(RA, 20 * KB, 4 * KB, BF16)
        jm4 = [view(RD, t * 4 * KB, 4 * KB, BF16) for t in range(4)]
        wouta = view(RD, 16 * KB, 8 * KB, BF16).rearrange("p (h d) -> p h d", h=4)
        attnT = view(RD, 24 * KB, 2 * KB, BF16).rearrange("p (h t) -> p h t", h=4)
        attn_tm = view(RD, 26 * KB, 2 * KB, BF16).rearrange("p (t c) -> p t c", t=2)
        pT = [view(RD, 28 * KB + i * KB, KB, BF16) for i in range(4)]
        S.add("pool", lambda e: e.dma_start(out=wouta[:], in_=wout_d[0:512, :].rearrange("(h p) d -> p h d", p=128)),
              writes=["wouta"], dma=True)
        trib = identb[:, 128:256]
        CAND, CNT, TT, MN, MX, TH = 0, 1, 2, 3, 4, 5
        pt_ctr = [0]

        def gen_A(qb):
            if qb == 0:
                j0, j1 = jm4[0], jm4[1]
                S.add("dve", lambda e: e.tensor_copy(out=j0[:, 0:128], in_=cbias), reads=["cst"], writes=[("jm", 0, 0)])
                S.add("dve", lambda e: e.memset(j1[:, 0:128], 0.0), writes=[("jm", 0, 1)])
                S.add("dve", lambda e: e.tensor_copy(out=j1[:, 128:256], in_=cbias), reads=["cst"], writes=[("jm", 0, 1)])
                return
            def tile_vars(tl):
                tq = 2 * qb + tl
                n = (tq + 1) * 128
                nchunk = (n + 511) // 512
                return tq, n, nchunk, accb[tl], jm4[(qb % 2) * 2 + tl], bis[:, tl * 8:(tl + 1) * 8], "b%d" % tl

            def stage_scores(tl):
                tq, n, nchunk, acc, jm, bb, bn = tile_vars(tl)
                for h in range(4):
                    hp, par = h // 2, h % 2
                    for sc in range(nchunk):
                        w = min(512, n - sc * 512)
                        pi = (h * nchunk + sc) % 2
                        S.add("pe", lambda e, hp=hp, par=par, sc=sc, w=w, pi=pi, tq=tq: e.matmul(
                            PS[pi][:, 0:w], lhsT=qiT[par * 64:(par + 1) * 64, hp, tq * 128:(tq + 1) * 128],
                            rhs=kiT2[par * 64:(par + 1) * 64, sc * 512:sc * 512 + w], start=True, stop=True),
                            reads=[("qiT", tq)] + [("kiT2", t) for t in range(sc * 4, sc * 4 + (w + 127) // 128)],
                            writes=[("ps", pi)])
                        S.add("act", lambda e, w=w, pi=pi, tq=tq, h=h: e.activation(
                            out=PS[pi][:, 0:w], in_=PS[pi][:, 0:w], func=AF.Relu, scale=wabs[:, tq, h:h + 1]),
                            reads=[("ps", pi), "wabs"], writes=[("ps", pi)])
                        if h == 0:
                            S.add("dve", lambda e, sc=sc, w=w, tq=tq, pi=pi, acc=acc: e.tensor_scalar(
                                out=acc[:, sc * 512:sc * 512 + w], in0=PS[pi][:, 0:w],
                                scalar1=wsgn[:, tq, 0:1], scalar2=None, op0=ALU.mult),
                                reads=[("ps", pi), "wsgn"], writes=[("acc", tl, sc)])
                        else:
                            S.add("dve", lambda e, sc=sc, w=w, tq=tq, h=h, pi=pi, acc=acc: e.scalar_tensor_tensor(
                                out=acc[:, sc * 512:sc * 512 + w], in0=PS[pi][:, 0:w],
                                scalar=wsgn[:, tq, h:h + 1], in1=acc[:, sc * 512:sc * 512 + w],
                                op0=ALU.mult, op1=ALU.add),
                                reads=[("ps", pi), "wsgn", ("acc", tl, sc)], writes=[("acc", tl, sc)])

            def stage_setup(tl):
                tq, n, nchunk, acc, jm, bb, bn = tile_vars(tl)
                accs = [("acc", tl, sc) for sc in range(nchunk)]
                S.add("dve", lambda e, n=n, acc=acc, bb=bb: e.tensor_reduce(out=bb[:, MN:MN + 1], in_=acc[:, 0:n], axis=AX.X, op=ALU.min),
                      reads=accs, writes=[bn + "mn"])
                S.add("dve", lambda e, tq=tq, acc=acc: e.tensor_tensor(out=acc[:, tq * 128:(tq + 1) * 128],
                                                                      in0=acc[:, tq * 128:(tq + 1) * 128], in1=cbias, op=ALU.add),
                      reads=accs + ["cst", bn + "mn"], writes=accs)
                S.add("dve", lambda e, n=n, acc=acc, bb=bb: e.tensor_reduce(out=bb[:, MX:MX + 1], in_=acc[:, 0:n], axis=AX.X, op=ALU.max),
                      reads=accs, writes=[bn + "mx"])
                S.add("dve", lambda e, bb=bb: e.tensor_tensor(out=bb[:, MX:MX + 1], in0=bb[:, MX:MX + 1], in1=bb[:, MN:MN + 1],
                                                              op=ALU.subtract), reads=[bn + "mx", bn + "mn"], writes=[bn + "rng"])
                S.add("dve", lambda e, bb=bb: e.tensor_scalar(out=bb[:, MX:MX + 1], in0=bb[:, MX:MX + 1], scalar1=1e-6,
                                                              scalar2=None, op0=ALU.max), reads=[bn + "rng"], writes=[bn + "rng2"])
                S.add("dve", lambda e, bb=bb: e.reciprocal(out=bb[:, MX:MX + 1], in_=bb[:, MX:MX + 1]),
                      reads=[bn + "rng2"], writes=[bn + "irng"])
                S.add("dve", lambda e, n=n, acc=acc, bb=bb: e.tensor_scalar(
                    out=acc[:, 0:n], in0=acc[:, 0:n], scalar1=bb[:, MN:MN + 1], scalar2=bb[:, MX:MX + 1],
                    op0=ALU.subtract, op1=ALU.mult),
                    reads=accs + [bn + "mn", bn + "irng"], writes=accs)
                S.add("dve", lambda e, bb=bb, tl=tl: e.memset(bb[:, CAND:CAND + 1], 0.5 if tl == 0 else -0.5),
                      writes=[bn + "cand"])

            def stage_iter(tl, it_):
                tq, n, nchunk, acc, jm, bb, bn = tile_vars(tl)
                accs = [("acc", tl, sc) for sc in range(nchunk)]
                if tl == 0:
                    S.add("dve", lambda e, n=n, acc=acc, bb=bb: e.tensor_scalar(
                        out=junkD[:, 0:n], in0=acc[:, 0:n], scalar1=bb[:, CAND:CAND + 1], scalar2=None,
                        op0=ALU.is_ge, op1=ALU.add, accum_out=bb[:, CNT:CNT + 1]),
                        reads=accs + [bn + "cand"], writes=[bn + "cnt"])
                    S.add("dve", lambda e, it_=it_, bb=bb: e.tensor_scalar(
                        out=bb[:, TT:TT + 1], in0=bb[:, CNT:CNT + 1], scalar1=TOPK - 0.5, scalar2=2.0 ** (-it_),
                        op0=ALU.is_ge, op1=ALU.mult), reads=[bn + "cnt"], writes=[bn + "tt"])
                    S.add("dve", lambda e, it_=it_, bb=bb: e.scalar_tensor_tensor(
                        out=bb[:, CAND:CAND + 1], in0=bb[:, TT:TT + 1], scalar=-(2.0 ** (-(it_ + 1))),
                        in1=bb[:, CAND:CAND + 1], op0=ALU.add, op1=ALU.add),
                        reads=[bn + "tt", bn + "cand"], writes=[bn + "cand"])
                else:
                    S.add("act", lambda e, n=n, acc=acc, bb=bb: e.activation(
                        out=junkA[:, 0:n], in_=acc[:, 0:n], func=AF.Sign, bias=bb[:, CAND:CAND + 1], scale=1.0,
                        accum_out=bb[:, CNT:CNT + 1]),
                        reads=accs + [bn + "cand"], writes=[bn + "cnt"])
                    S.add("act", lambda e, n=n, bb=bb: e.activation(
                        out=bb[:, TT:TT + 1], in_=bb[:, CNT:CNT + 1], func=AF.Sign, bias=float(n - 2 * TOPK + 1), scale=1.0),
                        reads=[bn + "cnt"], writes=[bn + "tt"])
                    S.add("act", lambda e, it_=it_, bb=bb: e.activation(
                        out=bb[:, CAND:CAND + 1], in_=bb[:, TT:TT + 1], func=AF.Identity,
                        bias=bb[:, CAND:CAND + 1], scale=-(2.0 ** (-(it_ + 1)))),
                        reads=[bn + "tt", bn + "cand"], writes=[bn + "cand"])

            def stage_mask(tl):
                tq, n, nchunk, acc, jm, bb, bn = tile_vars(tl)
                accs = [("acc", tl, sc) for sc in range(nchunk)]
                if tl == 0:
                    S.add("dve", lambda e, bb=bb: e.tensor_scalar(out=bb[:, TH:TH + 1], in0=bb[:, CAND:CAND + 1],
                                                                  scalar1=-(2.0 ** (-(NBIS + 1))), scalar2=None, op0=ALU.add),
                          reads=[bn + "cand"], writes=[bn + "th"])
                else:
                    S.add("dve", lambda e, bb=bb: e.tensor_scalar(out=bb[:, TH:TH + 1], in0=bb[:, CAND:CAND + 1],
                                                                  scalar1=-1.0, scalar2=-(2.0 ** (-(NBIS + 1))),
                                                                  op0=ALU.mult, op1=ALU.add),
                          reads=[bn + "cand"], writes=[bn + "th"])
                S.add("dve", lambda e, n=n, acc=acc, bb=bb, jm=jm: e.tensor_scalar(
                    out=jm[:, 0:n], in0=acc[:, 0:n], scalar1=bb[:, TH:TH + 1], scalar2=bis[:, 23:24],
                    op0=ALU.is_lt, op1=ALU.mult),
                    reads=accs + [bn + "th", "negbig"], writes=[("jm", qb % 2, tl)])

            for tl in range(2):
                stage_scores(tl)
                stage_setup(tl)
            for it_ in range(1, NBIS + 1):
                stage_iter(0, it_)
                stage_iter(1, it_)
            for tl in range(2):
                stage_mask(tl)

        def gen_B(qb):
            npair = qb + 1
            units = [(h, p) for h in range(8) for p in range(npair)]

            def front(k):
                h, p = units[k]
                hp, par = h // 2, h % 2
                pS = 2 + k % 2
                pti = k % 4
                for j in range(2):
                    sbk = 2 * p + j
                    c0 = 128 if sbk == 2 * qb + 1 else 0
                    S.add("pe", lambda e, hp=hp, par=par, sbk=sbk, c0=c0, pS=pS, j=j: e.matmul(
                        PS[pS][:, j * QB + c0:(j + 1) * QB], lhsT=kT[par * 64:(par + 1) * 64, hp, sbk * 128:(sbk + 1) * 128],
                        rhs=qT[par * 64:(par + 1) * 64, hp, qb * QB + c0:(qb + 1) * QB], start=(j == 0), stop=False,
                        skip_group_check=True),
                        reads=[("kT", sbk)] + [("qT", 2 * qb + t) for t in range(2)], writes=[("ps", pS)])
                    for tl in range(c0 // 128, 2):
                        jm = jm4[(qb % 2) * 2 + tl]
                        S.add("pe", lambda e, sbk=sbk, tl=tl, pS=pS, jm=jm, j=j: e.matmul(
                            PS[pS][:, j * QB + tl * 128:j * QB + (tl + 1) * 128], lhsT=jm[:, sbk * 128:(sbk + 1) * 128],
                            rhs=identb[:, 0:128], start=False, stop=(j == 1 and tl == 1), skip_group_check=True),
                            reads=[("jm", qb % 2, tl), "identb"], writes=[("ps", pS)])
                S.add("act", lambda e, pS=pS, pti=pti: e.activation(
                    out=pT[pti][:], in_=PS[pS][:], func=AF.Exp, scale=0.125),
                    reads=[("ps", pS)], writes=[("pT", pti)])

            def back(k):
                h, p = units[k]
                po = 4 + h % 2
                pti = k % 4
                for j in range(2):
                    sbk = 2 * p + j
                    c0 = 128 if sbk == 2 * qb + 1 else 0
                    for tl in range(c0 // 128, 2):
                        first = (sbk == 0 and tl == 0)
                        last = (sbk == 2 * qb + tl)
                        S.add("pe", lambda e, sbk=sbk, h=h, tl=tl, po=po, pti=pti, first=first, last=last, j=j: e.matmul(
                            PS[po][:, tl * 65:(tl + 1) * 65], lhsT=pT[pti][:, j * QB + tl * 128:j * QB + (tl + 1) * 128],
                            rhs=Vt[:, sbk, h, :], start=first, stop=last, skip_group_check=True),
                            reads=[("Vt", sbk), "V", ("pT", pti)], writes=[("ps", po)])
                if p == npair - 1:
                    pv = PS[po][:, 0:130].rearrange("p (t c) -> p t c", c=65)
                    rd = bis[:, 16 + 2 * (h % 2):18 + 2 * (h % 2)]
                    S.add("dve", lambda e, pv=pv, rd=rd: e.reciprocal(out=rd, in_=pv[:, :, 64]),
                          reads=[("ps", po)], writes=[("rden", h % 2)])
                    S.add("dve", lambda e, h=h, pv=pv, rd=rd: e.tensor_tensor(
                        out=attn_tm[:, :, h * 64:(h + 1) * 64], in0=pv[:, :, 0:64],
                        in1=rd.unsqueeze(2).to_broadcast([128, 2, 64]), op=ALU.mult),
                        reads=[("ps", po), ("rden", h % 2)], writes=["attn_tm"])

            front(0)
            for k in range(len(units)):
                if k + 1 < len(units):
                    front(k + 1)
                back(k)
            for tl in range(2):
                i = 2 * qb + tl
                for hp in range(4):
                    S.add("pe", lambda e, tl=tl, hp=hp: e.transpose(
                        out=psT_bf(7)[:, hp * 128:(hp + 1) * 128], in_=attn_tm[:, tl, hp * 128:(hp + 1) * 128],
                        identity=identb[:, 0:128]),
                        reads=["attn_tm", "identb"], writes=[("ps", 7)])
                S.add("act", lambda e, tl=tl: e.activation(
                    out=attnT[:, :, tl * 128:(tl + 1) * 128],
                    in_=psT_bf(7)[:, 0:512].rearrange("p (h t) -> p h t", h=4), func=AF.Copy),
                    reads=[("ps", 7)], writes=["attnT"])
                for dh in range(2):
                    for hp in range(4):
                        S.add("pe", lambda e, tl=tl, dh=dh, hp=hp: e.matmul(
                            PS[7][:], lhsT=attnT[:, hp, tl * 128:(tl + 1) * 128],
                            rhs=wouta[:, hp, dh * 512:(dh + 1) * 512], start=(hp == 0), stop=(hp == 3)),
                            reads=["attnT", "wouta"], writes=[("ps", 7)])
                    S.add("dve", lambda e, i=i, dh=dh: e.tensor_tensor(
                        out=resid[:, i, dh * 512:(dh + 1) * 512], in0=PS[7][:], in1=resid[:, i, dh * 512:(dh + 1) * 512],
                        op=ALU.add),
                        reads=[("ps", 7), ("resid", i, dh)], writes=[("resid", i, dh)])

        gen_A(0)
        for qb in range(NQB):
            S.begin_capture()
            gen_B(qb)
            lb = S.end_capture()
            la = []
            if qb + 1 < NQB:
                S.begin_capture()
                gen_A(qb + 1)
                la = S.end_capture()
            S.merge([lb, la])

        dead = S.retire(lambda r: (isinstance(r, tuple) and r[0] in ("acc", "jm", "maskT", "pT", "qT", "kT", "Vt", "qiT", "kiT2"))
                        or r in ("attnT", "attn_tm", "wouta", "rden", "V"))
        for nm in ([("xnT", i) for i in range(NT)] + ["grep", ("xn_bf", 0), ("xn_bf", 1), ("wdb", 0), ("wdb", 1),
                   ("wgb", 0), ("wgb", 1), ("wub", 0), ("wub", 1), ("sgb", 0), ("sgb", 1)]
                   + [("hT", c, t) for c in range(6) for t in range(4)]):
            S.seed(nm, dead)
        ffn(1, 2, False, True)
        S.add("sp", lambda e: e.nop(), reads=[("out", i) for i in range(NT)])
        S.emit(st)
    return nc


def _consts():
    c = np.zeros((128, NCST), np.float32)
    p = np.arange(128)
    c[:, O_CB:O_CB + 128] = np.where(p[None, :] <= p[:, None], 0.0, NEG).astype(np.float32)
    pos = (np.arange(NT)[None, :] * 128 + p[:, None]).astype(np.float32)
    for half, oc, os_ in ((32, O_COSQ, O_SINQ), (16, O_COSI, O_SINI)):
        inv = (np.float32(10000.0) ** (-np.arange(half, dtype=np.float32) / np.float32(half))).astype(np.float32)
        ang = (pos[:, :, None] * inv[None, None, :]).astype(np.float32)
        c[:, oc:oc + NT * half] = np.cos(ang).astype(np.float32).reshape(128, -1)
        c[:, os_:os_ + NT * half] = np.sin(ang).astype(np.float32).reshape(128, -1)
    c[:, O_INVC:O_INVC + 16] = (1.0 / np.arange(1, 17, dtype=np.float32))[None, :]
    return c


_PROG = None


def kernel(x, ffn1_norm, ffn1_w_gate, ffn1_w_up, ffn1_w_down, mix_norm, w_in, q_norm, k_norm,
           pool_w, pool_scale, w_out, ffn2_norm, ffn2_w_gate, ffn2_w_up, ffn2_w_down):
    global _PROG
    f = lambda a: np.ascontiguousarray(np.asarray(a, dtype=np.float32))
    x = f(x)
    cst = _consts()
    cst[:, O_G1:O_G1 + 8] = f(ffn1_norm)[0].reshape(8, 128).T
    cst[:, O_GM:O_GM + 8] = f(mix_norm)[0].reshape(8, 128).T
    cst[:, O_G2:O_G2 + 8] = f(ffn2_norm)[0].reshape(8, 128).T
    cst[:, O_GQ:O_GQ + 64] = f(q_norm)[0][None, :]
    cst[:, O_GK:O_GK + 64] = f(k_norm)[0][None, :]
    cst[:, O_PS:O_PS + 4] = f(pool_scale)[0].reshape(4, 128).T
    shared = {
        "ffn1_w_gate": f(ffn1_w_gate)[0], "ffn2_w_gate": f(ffn2_w_gate)[0],
        "ffn1_w_up": f(ffn1_w_up)[0], "ffn2_w_up": f(ffn2_w_up)[0],
        "ffn1_w_down": f(ffn1_w_down)[0], "ffn2_w_down": f(ffn2_w_down)[0],
        "w_in": f(w_in)[0], "w_out": f(w_out)[0], "pool_w": f(pool_w)[0],
        "grep": np.ascontiguousarray(np.broadcast_to(
            np.stack([f(ffn1_norm)[0], f(mix_norm)[0], f(ffn2_norm)[0]])[:, None, :], (3, 128, DM))),
        "cst": cst, "ident": np.concatenate([np.eye(128, dtype=np.float32), np.triu(np.ones((128, 128), np.float32))], axis=1),
    }
    if _PROG is None:
        _PROG = build_program()
    n = x.shape[0]
    in_maps = [dict(shared, x=x[b]) for b in range(n)]
    res = run_bass_kernel_spmd(_PROG, in_maps, core_ids=list(range(n)))
    return np.stack([np.asarray(r["out"], dtype=np.float32) for r in res.results], axis=0)
```

```python
import math
from contextlib import ExitStack
import numpy as np
import concourse.bass as bass
import concourse.mybir as mybir
from concourse.bass_utils import run_bass_kernel_spmd

F32 = mybir.dt.float32
BF16 = mybir.dt.bfloat16
AF = mybir.ActivationFunctionType
ALU = mybir.AluOpType
AX = mybir.AxisListType

SEQ = 2048
DM = 1024
DFF = 2816
NT = SEQ // 128
NCH = DFF // 128
EPS = 1e-6
TOPK = 256
NBIS = 16
NEG = -1.0e30
CQ, CK, CV, CQI, CKI, CWI, CU = 0, 512, 1024, 1536, 1792, 1856, 1860
O_CB, O_COSQ, O_SINQ, O_COSI, O_SINI, O_INVC = 0, 128, 640, 1152, 1408, 1664
O_G1, O_GM, O_G2, O_GQ, O_GK, O_PS = 1680, 1688, 1696, 1704, 1768, 1832
NCST = 1836


class _Op:
    __slots__ = ("eng", "fn", "deps", "idx", "is_dma", "sem", "val", "milestone")


class Sched:
    ENG = ("pe", "act", "dve", "pool", "sp")
    ROT = 900

    def __init__(self, nc, n_dma_sems=20):
        self.nc = nc
        self.ops = []
        self.last_w = {}
        self.readers = {}
        self.n_dma_sems = n_dma_sems
        self.cap = None

    def add(self, eng, fn, reads=(), writes=(), dma=False):
        if self.cap is not None:
            self.cap.append((eng, fn, tuple(reads), tuple(writes), dma))
            return None
        op = _Op()
        op.eng = eng; op.fn = fn; op.is_dma = dma; op.idx = len(self.ops)
        op.milestone = False; op.sem = None; op.val = 0
        deps = set()
        for r in reads:
            w = self.last_w.get(r)
            if w is not None:
                deps.add(w)
        for r in writes:
            w = self.last_w.get(r)
            if w is not None:
                deps.add(w)
            for rd in self.readers.get(r, ()):
                deps.add(rd)
        op.deps = sorted(deps)
        for r in reads:
            self.readers.setdefault(r, []).append(op.idx)
        for r in writes:
            self.last_w[r] = op.idx
            self.readers[r] = []
        self.ops.append(op)
        return op.idx

    def begin_capture(self):
        self.cap = []

    def end_capture(self):
        c = self.cap
        self.cap = None
        return c

    def merge(self, lists):
        lists = [l for l in lists if l]
        pos = [0] * len(lists)
        while True:
            best, bf = None, None
            for k, l in enumerate(lists):
                if pos[k] < len(l):
                    f = pos[k] / len(l)
                    if bf is None or f < bf:
                        best, bf = k, f
            if best is None:
                break
            self.add(*lists[best][pos[best]])
            pos[best] += 1

    def retire(self, pred):
        out = set()
        for r in list(self.last_w.keys()):
            if pred(r):
                out.add(self.last_w.pop(r))
        for r in list(self.readers.keys()):
            if pred(r):
                out.update(self.readers.pop(r))
        return out

    def seed(self, name, opset):
        self.readers.setdefault(name, []).extend(sorted(opset))

    def emit(self, stack):
        nc = self.nc
        ops = self.ops
        for op in ops:
            for d in op.deps:
                p = ops[d]
                if p.eng == "pe" and op.eng == "pe" and not p.is_dma and not op.is_dma:
                    continue
                p.milestone = True
        n_ms = {e: 0 for e in self.ENG}
        for op in ops:
            if op.milestone and not op.is_dma:
                n_ms[op.eng] += 1
        esems = {e: [stack.enter_context(nc.semaphore("s_%s%d" % (e, i)))
                     for i in range(n_ms[e] // self.ROT + 1)] for e in self.ENG}
        dsems = [stack.enter_context(nc.semaphore("d%d" % i)) for i in range(self.n_dma_sems)]
        ecount = {e: 0 for e in self.ENG}
        dcount = [0] * self.n_dma_sems
        dlast = [None] * self.n_dma_sems
        dnext = {e: 0 for e in self.ENG}
        prev_on_dsem = {}
        for op in ops:
            if op.is_dma:
                half = self.n_dma_sems // 2
                base = 0 if op.eng == "pool" else half
                j = base + dnext[op.eng] % half
                dnext[op.eng] += 1
                if dlast[j] is not None:
                    prev_on_dsem[op.idx] = dlast[j]
                dcount[j] += 16
                op.sem = dsems[j]; op.val = dcount[j]; dlast[j] = op.idx
                op.milestone = True
            elif op.milestone:
                c = ecount[op.eng]
                ecount[op.eng] += 1
                op.sem = esems[op.eng][c // self.ROT]; op.val = c % self.ROT + 1
        per_eng = {e: [o for o in ops if o.eng == e] for e in self.ENG}
        block = stack.enter_context(nc.Block())

        def run(engname, eng):
            waited = {}
            for op in per_eng[engname]:
                deps = list(op.deps)
                if op.idx in prev_on_dsem:
                    deps.append(prev_on_dsem[op.idx])
                need = {}
                for d in deps:
                    p = ops[d]
                    if p.eng == "pe" and op.eng == "pe" and not p.is_dma and not op.is_dma:
                        continue
                    key = id(p.sem)
                    if waited.get(key, 0) >= p.val:
                        continue
                    if key not in need or need[key][1] < p.val:
                        need[key] = (p.sem, p.val)
                for key, (sem, val) in need.items():
                    eng.wait_ge(sem, val)
                    waited[key] = val
                ins = op.fn(eng)
                if op.is_dma:
                    ins.then_inc(op.sem, 16)
                elif op.milestone:
                    ins.then_inc(op.sem, 1)

        @block.tensor
        def _(e):
            run("pe", e)

        @block.scalar
        def _(e):
            run("act", e)

        @block.vector
        def _(e):
            run("dve", e)

        @block.gpsimd
        def _(e):
            run("pool", e)

        @block.sync
        def _(e):
            run("sp", e)


def build_program():
    nc = bass.Bass("TRN2", target_bir_lowering=False)

    def din(name, shape):
        return nc.dram_tensor(name, shape, F32, kind="ExternalInput").ap()

    x_d = din("x", [SEQ, DM])
    wg_d = [din("ffn1_w_gate", [DM, DFF]), din("ffn2_w_gate", [DM, DFF])]
    wu_d = [din("ffn1_w_up", [DM, DFF]), din("ffn2_w_up", [DM, DFF])]
    wd_d = [din("ffn1_w_down", [DFF, DM]), din("ffn2_w_down", [DFF, DM])]
    win_d = din("w_in", [DM, 2372])
    wout_d = din("w_out", [DM, DM])
    poolw_d = din("pool_w", [4, 128, 128])
    cst_d = din("cst", [128, NCST])
    ident_d = din("ident", [128, 256])
    grep_d = din("grep", [3, 128, DM])
    out_d = nc.dram_tensor("out", [SEQ, DM], F32, kind="ExternalOutput").ap()

    with ExitStack() as st:
        def sb(name, shape, dt=F32):
            return st.enter_context(nc.sbuf_tensor(name, shape, dt))

        resid = sb("resid", [128, NT, DM], F32)
        RA = sb("RA", [128, 16384], BF16)
        RC = sb("RC", [128, 31232], BF16)
        RD = sb("RD", [128, 20480], BF16)
        cst = sb("cst_sb", [128, NCST], F32)
        identb = sb("identb", [128, 256], BF16)
        ones = sb("ones", [128, 64], F32)
        ss = sb("ss", [128, NT], F32)
        rstd = sb("rstd", [128, NT], F32)
        ss8 = sb("ss8", [128, 2, 8], F32)
        rs8 = sb("rs8", [128, 2, 8], F32)
        wraw = sb("wraw", [128, NT, 4], F32)
        wabs = sb("wabs", [128, NT, 4], F32)
        wsgn = sb("wsgn", [128, NT, 4], F32)
        bis = sb("bis", [128, 24], F32)
        PS = [st.enter_context(nc.psum_tensor("ps%d" % i, [128, 512], F32)) for i in range(8)]

        def view(reg, off_b, nbytes, dt):
            v = reg[:, off_b // 2:(off_b + nbytes) // 2]
            return v if dt == BF16 else v.bitcast(dt)

        KB = 1024
        S = Sched(nc)

        S.add("sp", lambda e: e.dma_start(out=cst[:], in_=cst_d[:]), writes=["cst"], dma=True)
        S.add("pool", lambda e: e.dma_start(out=identb[:], in_=ident_d[:]), writes=["identb"], dma=True)
        S.add("dve", lambda e: e.memset(ones[:], 1.0), writes=["ones"])
        S.add("dve", lambda e: e.memset(bis[:, 23:24], -30000.0), writes=["negbig"])
        cbias = cst[:, O_CB:O_CB + 128]

        def gT(off):
            return cst[:, off:off + 8]

        def psT_bf(k):
            return PS[k][:].bitcast(BF16)

        xn_bf = [view(RD, 16 * KB + i * 2 * KB, 2 * KB, BF16) for i in range(2)]
        xnT = RA[:].rearrange("p (k t) -> p k t", k=8)
        g_rep = view(RD, 24 * KB, 4 * KB, F32)

        def rms_blocks(gidx, load_x):
            S.add("sp", lambda e: e.dma_start(out=g_rep[:], in_=grep_d[gidx]), writes=["grep"], dma=True)
            stats, norms = [], []
            for b in range(4):
                bs = slice(4 * b, 4 * b + 4)
                S.begin_capture()
                S.add("dve", lambda e, bs=bs: e.memset(ss[:, bs], 0.0), writes=[("ss", b)])
                for i in range(4 * b, 4 * b + 4):
                    if load_x:
                        S.add("sp", lambda e, i=i: e.dma_start(out=resid[:, i, :], in_=x_d[i * 128:(i + 1) * 128, :]),
                              writes=[("resid", i, 0), ("resid", i, 1)], dma=True)
                    S.add("act", lambda e, i=i: e.activation(out=xn_bf[i % 2][:], in_=resid[:, i, :], func=AF.Square,
                                                             accum_out=ss[:, i:i + 1]),
                          reads=[("resid", i, 0), ("resid", i, 1), ("ss", b)], writes=[("xn_bf", i % 2), ("ssv", i)])
                S.add("act", lambda e, bs=bs: e.activation(out=rstd[:, bs], in_=ss[:, bs], func=AF.Sqrt, scale=1.0 / DM, bias=EPS),
                      reads=[("ssv", i) for i in range(4 * b, 4 * b + 4)] + [("ss", b)], writes=[("rstd0", b)])
                S.add("dve", lambda e, bs=bs: e.reciprocal(out=rstd[:, bs], in_=rstd[:, bs]),
                      reads=[("rstd0", b)], writes=[("rstd", b)])
                stats.append(S.end_capture())
                S.begin_capture()
                for i in range(4 * b, 4 * b + 4):
                    S.add("dve", lambda e, i=i: e.scalar_tensor_tensor(
                        out=xn_bf[i % 2][:], in0=resid[:, i, :], scalar=rstd[:, i:i + 1], in1=g_rep[:],
                        op0=ALU.mult, op1=ALU.mult),
                        reads=[("resid", i, 0), ("resid", i, 1), ("rstd", b), "grep"], writes=[("xn_bf", i % 2)])
                    pk = 6 + (i % 2)
                    for k in range(8):
                        S.add("pe", lambda e, i=i, k=k, pk=pk: e.transpose(
                            out=psT_bf(pk)[:, k * 128:(k + 1) * 128], in_=xn_bf[i % 2][:, k * 128:(k + 1) * 128],
                            identity=identb[:, 0:128]),
                            reads=[("xn_bf", i % 2), "identb"], writes=[("ps", pk)])
                    S.add("act", lambda e, i=i, pk=pk: e.activation(
                        out=xnT[:, :, i * 128:(i + 1) * 128],
                        in_=psT_bf(pk).rearrange("p (k t) -> p k t", k=8), func=AF.Copy),
                        reads=[("ps", pk)], writes=[("xnT", i)])
                norms.append(S.end_capture())
            return stats, norms

        def emit_norm_interleaved(sn, body, n_head, unit):
            stats, norms = sn
            seq = [body[:n_head], stats[0], stats[1], norms[0], body[n_head:n_head + unit],
                   stats[2], norms[1], body[n_head + unit:n_head + 2 * unit],
                   stats[3], norms[2], body[n_head + 2 * unit:n_head + 3 * unit],
                   norms[3], body[n_head + 3 * unit:]]
            for l in seq:
                for o in l:
                    S.add(*o)

        GROUPS = [(0, 6), (6, 6), (12, 6), (18, 4)]
        hT = view(RC, 0, 24 * KB, BF16).rearrange("p (c t) -> p c t", c=6)
        wdb = [view(RC, 24 * KB + i * 12 * KB, 12 * KB, BF16).rearrange("p (c d) -> p c d", c=6) for i in range(2)]
        wgb = [view(RD, i * 4 * KB, 4 * KB, BF16).rearrange("p (k f) -> p k f", k=8) for i in range(2)]
        wub = [view(RD, 8 * KB + i * 4 * KB, 4 * KB, BF16).rearrange("p (k f) -> p k f", k=8) for i in range(2)]
        sgb = [view(RD, 20 * KB + i * 2 * KB, 2 * KB, F32) for i in range(2)]

        def ffn(li, goff, load_x, final):
            sn = rms_blocks(goff, load_x)
            S.begin_capture()
            pair_ctr = [0]
            for gi, (c0, ncg) in enumerate(GROUPS):
                wd_t = wdb[gi % 2]

                def _wd_dma(c0=c0, ncg=ncg, wd_t=wd_t, gi=gi):
                    S.add("pool", lambda e: e.dma_start(
                        out=wd_t[:, 0:ncg, :],
                        in_=wd_d[li][c0 * 128:(c0 + ncg) * 128, :].rearrange("(c p) d -> p c d", p=128)),
                        writes=[("wdb", gi % 2)], dma=True)
                for pr in range(ncg // 2):
                    j = pair_ctr[0] % 2
                    pair_ctr[0] += 1
                    col0 = (c0 + 2 * pr) * 128
                    S.add("pool", lambda e, j=j, col0=col0: e.dma_start(
                        out=wgb[j][:], in_=wg_d[li][:, col0:col0 + 256].rearrange("(k p) f -> p k f", p=128)),
                        writes=[("wgb", j)], dma=True)
                    S.add("pool", lambda e, j=j, col0=col0: e.dma_start(
                        out=wub[j][:], in_=wu_d[li][:, col0:col0 + 256].rearrange("(k p) f -> p k f", p=128)),
                        writes=[("wub", j)], dma=True)
                    if pr == 0:
                        _wd_dma()
                    for cc in range(2):
                        cl = 2 * pr + cc
                        for tb in range(4):
                            q = (cl * 4 + tb) % 2
                            pg, pu = q, 2 + q
                            for k in range(8):
                                S.add("pe", lambda e, j=j, cc=cc, tb=tb, k=k, pg=pg: e.matmul(
                                    PS[pg][:], lhsT=wgb[j][:, k, cc * 128:(cc + 1) * 128],
                                    rhs=xnT[:, k, tb * 512:(tb + 1) * 512], start=(k == 0), stop=(k == 7)),
                                    reads=[("wgb", j)] + [("xnT", 4 * tb + t) for t in range(4)], writes=[("ps", pg)])
                            for k in range(8):
                                S.add("pe", lambda e, j=j, cc=cc, tb=tb, k=k, pu=pu: e.matmul(
                                    PS[pu][:], lhsT=wub[j][:, k, cc * 128:(cc + 1) * 128],
                                    rhs=xnT[:, k, tb * 512:(tb + 1) * 512], start=(k == 0), stop=(k == 7)),
                                    reads=[("wub", j)] + [("xnT", 4 * tb + t) for t in range(4)], writes=[("ps", pu)])
                            S.add("act", lambda e, q=q, pg=pg: e.activation(out=sgb[q][:], in_=PS[pg][:], func=AF.Silu),
                                  reads=[("ps", pg)], writes=[("sgb", q)])
                            S.add("dve", lambda e, q=q, pu=pu, cl=cl, tb=tb: e.tensor_tensor(
                                out=hT[:, cl, tb * 512:(tb + 1) * 512], in0=sgb[q][:], in1=PS[pu][:], op=ALU.mult),
                                reads=[("sgb", q), ("ps", pu)], writes=[("hT", cl, tb)])
                for i in range(NT):
                    for dh in range(2):
                        pd = 4 + (i * 2 + dh) % 2
                        for cl in range(ncg):
                            S.add("pe", lambda e, i=i, dh=dh, cl=cl, pd=pd, wd_t=wd_t, ncg=ncg: e.matmul(
                                PS[pd][:], lhsT=hT[:, cl, i * 128:(i + 1) * 128],
                                rhs=wd_t[:, cl, dh * 512:(dh + 1) * 512], start=(cl == 0), stop=(cl == ncg - 1)),
                                reads=[("hT", cl, i // 4), ("wdb", gi % 2)], writes=[("ps", pd)])
                        S.add("dve", lambda e, i=i, dh=dh, pd=pd: e.scalar_tensor_tensor(
                            out=resid[:, i, dh * 512:(dh + 1) * 512], in0=PS[pd][:], scalar=0.5,
                            in1=resid[:, i, dh * 512:(dh + 1) * 512], op0=ALU.mult, op1=ALU.add),
                            reads=[("ps", pd), ("resid", i, dh)], writes=[("resid", i, dh)])
                    if final and gi == len(GROUPS) - 1:
                        S.add("sp", lambda e, i=i: e.dma_start(out=out_d[i * 128:(i + 1) * 128, :], in_=resid[:, i, :]),
                              reads=[("resid", i, 0), ("resid", i, 1)], writes=[("out", i)], dma=True)
            body = S.end_capture()
            emit_norm_interleaved(sn, body, 3, 18)

        ffn(0, 0, True, False)

        mix_sn = rms_blocks(1, False)
        dead = S.retire(lambda r: isinstance(r, tuple) and r[0] in ("hT", "wdb", "wgb", "wub", "sgb"))
        for nm in ["ubr", "wst0", "wst1", "mixscr"]:
            S.seed(nm, dead)
        S.begin_capture()

        UW = 16 + SEQ
        ubuf = [view(RC, i * 8256, 8256, F32) for i in range(3)]
        p2T = view(RC, 25 * KB, 16 * KB, BF16).rearrange("p (g t) -> p g t", g=4)
        woutp = view(RC, 41 * KB, 8 * KB, BF16).rearrange("p (g d) -> p g d", g=4)
        plT = view(RC, 49 * KB, 4 * KB, BF16)
        poolw = view(RC, 53 * KB, 1 * KB, BF16).rearrange("p (g d) -> p g d", g=4)
        wst = [view(RD, i * 8 * KB, 8 * KB, BF16).rearrange("p (k f) -> p k f", k=8) for i in range(2)]
        S.add("pool", lambda e: e.dma_start(out=wst[0][:], in_=win_d[:, CU:CU + 512].rearrange("(k p) f -> p k f", p=128)),
              writes=["wst0"], dma=True)
        S.add("pool", lambda e: e.dma_start(out=woutp[:], in_=wout_d[512:1024, :].rearrange("(g p) d -> p g d", p=128)),
              writes=["ubr"], dma=True)
        S.add("pool", lambda e: e.dma_start(out=poolw[:], in_=poolw_d.rearrange("g c d -> c g d")),
              writes=["ubr"], dma=True)
        for b in range(3):
            S.add("dve", lambda e, b=b: e.memset(ubuf[b][:, 0:16], 0.0), writes=["ubr"])
        for g in range(4):
            u, sa, sbf = ubuf[0], ubuf[1], ubuf[2]
            for tb in range(4):
                pp = tb % 2
                for k in range(8):
                    S.add("pe", lambda e, g=g, tb=tb, k=k, pp=pp: e.matmul(
                        PS[pp][:], lhsT=wst[0][:, k, g * 128:(g + 1) * 128], rhs=xnT[:, k, tb * 512:(tb + 1) * 512],
                        start=(k == 0), stop=(k == 7)),
                        reads=["wst0"] + [("xnT", 4 * tb + t) for t in range(4)], writes=[("ps", pp)])
                S.add("act", lambda e, tb=tb, pp=pp, u=u: e.activation(out=u[:, 16 + tb * 512:16 + (tb + 1) * 512],
                                                                        in_=PS[pp][:], func=AF.Copy),
                      reads=[("ps", pp)], writes=["ubr"])
            cur = u
            nxt = [sa, sbf]
            for stp in range(g + 1):
                sh = 1 << stp
                dst = nxt[stp % 2]
                S.add("dve", lambda e, cur=cur, dst=dst, sh=sh: e.tensor_tensor(
                    out=dst[:, 16:UW], in0=cur[:, 16:UW], in1=cur[:, 16 - sh:UW - sh], op=ALU.add),
                    reads=["ubr"], writes=["ubr"])
                cur = dst
            w = 2 << g
            S.add("dve", lambda e, cur=cur, u=u, w=w: e.scalar_tensor_tensor(
                out=plT[:], in0=cur[:, 16:UW], scalar=1.0 / w, in1=u[:, 16:UW], op0=ALU.mult, op1=ALU.subtract),
                reads=["ubr"], writes=["ubr"])
            tmpc = nxt[(g + 1) % 2]
            S.add("dve", lambda e, cur=cur, tmpc=tmpc, w=w: e.tensor_tensor(
                out=tmpc[:, 16:16 + w - 1], in0=cur[:, 16:16 + w - 1], in1=cst[:, O_INVC:O_INVC + w - 1], op=ALU.mult),
                reads=["ubr", "cst"], writes=["ubr"])
            S.add("dve", lambda e, tmpc=tmpc, u=u, w=w: e.tensor_tensor(
                out=plT[:, 0:w - 1], in0=tmpc[:, 16:16 + w - 1], in1=u[:, 16:16 + w - 1], op=ALU.subtract),
                reads=["ubr"], writes=["ubr"])
            for tb in range(4):
                pp = 2 + tb % 2
                S.add("pe", lambda e, g=g, tb=tb, pp=pp: e.matmul(
                    PS[pp][:], lhsT=poolw[:, g, :], rhs=plT[:, tb * 512:(tb + 1) * 512], start=True, stop=True),
                    reads=["ubr"], writes=[("ps", pp)])
                S.add("act", lambda e, g=g, tb=tb, pp=pp: e.activation(
                    out=p2T[:, g, tb * 512:(tb + 1) * 512], in_=PS[pp][:], func=AF.Copy,
                    scale=cst[:, O_PS + g:O_PS + g + 1]),
                    reads=[("ps", pp), "cst"], writes=["ubr"])
        for i in range(NT):
            for dh in range(2):
                pd = 4 + (i * 2 + dh) % 2
                for g in range(4):
                    S.add("pe", lambda e, i=i, dh=dh, g=g, pd=pd: e.matmul(
                        PS[pd][:], lhsT=p2T[:, g, i * 128:(i + 1) * 128], rhs=woutp[:, g, dh * 512:(dh + 1) * 512],
                        start=(g == 0), stop=(g == 3)),
                        reads=["ubr"], writes=[("ps", pd)])
                S.add("dve", lambda e, i=i, dh=dh, pd=pd: e.tensor_tensor(
                    out=resid[:, i, dh * 512:(dh + 1) * 512], in0=PS[pd][:], in1=resid[:, i, dh * 512:(dh + 1) * 512],
                    op=ALU.add),
                    reads=[("ps", pd), ("resid", i, dh)], writes=[("resid", i, dh)])
        ubody = S.end_capture()
        for l in (mix_sn[0][0], mix_sn[0][1], mix_sn[1][0], mix_sn[0][2], mix_sn[1][1], mix_sn[0][3], mix_sn[1][2], mix_sn[1][3], ubody):
            for o in l:
                S.add(*o)
        dead_g = S.retire(lambda r: r == "grep")
        for nm in ["qsq", "qn", "grep"]:
            S.seed(nm, dead_g)
        dead = S.retire(lambda r: r == "ubr")
        for nm in ["qT", "kT", "V", "qiT", "kiT2"]:
            S.seed(nm, dead)

        qT = view(RC, 0, 16 * KB, BF16).rearrange("p (h t) -> p h t", h=4)
        kT = view(RC, 16 * KB, 16 * KB, BF16).rearrange("p (h t) -> p h t", h=4)
        Vt = view(RC, 32 * KB, 16640, BF16).rearrange("p (i h d) -> p i h d", i=NT, h=8)
        qiT = view(RC, 32 * KB + 16640, 8 * KB, BF16).rearrange("p (h t) -> p h t", h=2)
        kiT2 = view(RC, 40 * KB + 16640, 4 * KB, BF16)
        MS = 20 * KB
        qraw = [view(RD, MS + i * 2 * KB, 2 * KB, F32) for i in range(2)]
        qsq = view(RD, MS + 4 * KB, 2 * KB, F32)
        qn = view(RD, MS + 6 * KB, 2 * KB, F32)
        rt = [view(RD, MS + 8 * KB + i * KB, KB, F32) for i in range(4)]
        qr = [view(RD, MS + 12 * KB + i * KB, KB, BF16) for i in range(2)]
        iraw = view(RD, MS + 14 * KB, 1296, F32)
        qib_l = [view(RD, MS + 14 * KB + 1296, 512, BF16), view(RD, MS + 14 * KB + 3088, 512, BF16)]
        ki2b_l = [view(RD, MS + 14 * KB + 1808, 256, BF16), view(RD, MS + 14 * KB + 3600, 256, BF16)]
        it = [view(RD, MS + 14 * KB + 2064 + i * 256, 256, F32) for i in range(4)]

        S.add("dve", lambda e: e.memset(Vt[:, :, :, 64:65], 1.0), writes=["V"])

        groups = [("q", CQ, 512), ("k", CK, 512), ("v", CV, 512), ("i", CQI, 324)]
        for gi, (gname, coff, ncol) in enumerate(groups):
            wj = (gi + 1) % 2
            S.add("pool", lambda e, wj=wj, coff=coff, ncol=ncol: e.dma_start(
                out=wst[wj][:, :, 0:ncol], in_=win_d[:, coff:coff + ncol].rearrange("(k p) f -> p k f", p=128)),
                writes=["wst%d" % wj], dma=True)
            tile_ops = []
            for i in range(NT):
                S.begin_capture()
                pp = i % 2
                for k in range(8):
                    S.add("pe", lambda e, i=i, k=k, pp=pp, wj=wj, ncol=ncol: e.matmul(
                        PS[pp][:, 0:ncol], lhsT=xnT[:, k, i * 128:(i + 1) * 128], rhs=wst[wj][:, k, 0:ncol],
                        start=(k == 0), stop=(k == 7)),
                        reads=["wst%d" % wj, ("xnT", i)], writes=[("ps", pp)])
                if gname in ("q", "k"):
                    goff = O_GQ if gname == "q" else O_GK
                    dstT = qT if gname == "q" else kT
                    qa = qraw[i % 2]
                    qrb = qr[i % 2]
                    q3 = qa.rearrange("p (h d) -> p h d", h=8)
                    S.add("act", lambda e, pp=pp, qa=qa: e.activation(out=qa[:], in_=PS[pp][:], func=AF.Copy),
                          reads=[("ps", pp)], writes=[("qraw", i % 2)])
                    S.add("dve", lambda e, qa=qa: e.tensor_tensor(out=qsq[:], in0=qa[:], in1=qa[:], op=ALU.mult),
                          reads=[("qraw", i % 2)], writes=["qsq"])
                    S.add("dve", lambda e, i=i: e.tensor_reduce(out=ss8[:, i % 2, :], in_=qsq.rearrange("p (h d) -> p h d", h=8),
                                                                axis=AX.X, op=ALU.add),
                          reads=["qsq"], writes=[("ss8", i % 2)])
                    S.add("act", lambda e, i=i: e.activation(out=rs8[:, i % 2, :], in_=ss8[:, i % 2, :], func=AF.Sqrt,
                                                             scale=1.0 / 64, bias=EPS),
                          reads=[("ss8", i % 2)], writes=[("rs8a", i % 2)])
                    qn3 = qn.rearrange("p (h d) -> p h d", h=8)
                    S.add("dve", lambda e, goff=goff, q3=q3: e.tensor_tensor(
                        out=qn3, in0=q3, in1=cst[:, goff:goff + 64].unsqueeze(1).to_broadcast([128, 8, 64]), op=ALU.mult),
                        reads=[("qraw", i % 2), "cst"], writes=["qn"])
                    S.add("dve", lambda e, i=i: e.reciprocal(out=rs8[:, i % 2, :], in_=rs8[:, i % 2, :]),
                          reads=[("rs8a", i % 2)], writes=[("rs8", i % 2)])
                    S.add("dve", lambda e, i=i: e.tensor_tensor(
                        out=qn3, in0=qn3, in1=rs8[:, i % 2, :].unsqueeze(2).to_broadcast([128, 8, 64]), op=ALU.mult),
                        reads=["qn", ("rs8", i % 2)], writes=["qn"])
                    cosb = cst[:, O_COSQ + i * 32:O_COSQ + (i + 1) * 32].unsqueeze(1).to_broadcast([128, 8, 32])
                    sinb = cst[:, O_SINQ + i * 32:O_SINQ + (i + 1) * 32].unsqueeze(1).to_broadcast([128, 8, 32])
                    r3 = [t.rearrange("p (h d) -> p h d", h=8) for t in rt]
                    qr3 = qrb.rearrange("p (h d) -> p h d", h=8)
                    x1, x2 = qn3[:, :, 0:32], qn3[:, :, 32:64]
                    S.add("dve", lambda e, x1=x1, cosb=cosb, r3=r3: e.tensor_tensor(out=r3[0], in0=x1, in1=cosb, op=ALU.mult),
                          reads=["qn", "cst"], writes=[("rt", 0)])
                    S.add("dve", lambda e, x2=x2, sinb=sinb, r3=r3: e.tensor_tensor(out=r3[1], in0=x2, in1=sinb, op=ALU.mult),
                          reads=["qn", "cst"], writes=[("rt", 1)])
                    S.add("dve", lambda e, r3=r3, qr3=qr3: e.tensor_tensor(out=qr3[:, :, 0:32], in0=r3[0], in1=r3[1], op=ALU.subtract),
                          reads=[("rt", 0), ("rt", 1)], writes=[("qr", i % 2)])
                    S.add("dve", lambda e, x2=x2, cosb=cosb, r3=r3: e.tensor_tensor(out=r3[2], in0=x2, in1=cosb, op=ALU.mult),
                          reads=["qn", "cst"], writes=[("rt", 2)])
                    S.add("dve", lambda e, x1=x1, sinb=sinb, r3=r3: e.tensor_tensor(out=r3[3], in0=x1, in1=sinb, op=ALU.mult),
                          reads=["qn", "cst"], writes=[("rt", 3)])
                    S.add("dve", lambda e, r3=r3, qr3=qr3: e.tensor_tensor(out=qr3[:, :, 32:64], in0=r3[2], in1=r3[3], op=ALU.add),
                          reads=[("rt", 2), ("rt", 3)], writes=[("qr", i % 2)])
                    pk = 6 + (i % 2)
                    for hp in range(4):
                        S.add("pe", lambda e, hp=hp, pk=pk, qrb=qrb: e.transpose(
                            out=psT_bf(pk)[:, hp * 128:(hp + 1) * 128], in_=qrb[:, hp * 128:(hp + 1) * 128],
                            identity=identb[:, 0:128]),
                            reads=[("qr", i % 2), "identb"], writes=[("ps", pk)])
                    S.add("act", lambda e, i=i, pk=pk, dstT=dstT: e.activation(
                        out=dstT[:, :, i * 128:(i + 1) * 128],
                        in_=psT_bf(pk)[:, 0:512].rearrange("p (h t) -> p h t", h=4), func=AF.Copy),
                        reads=[("ps", pk)], writes=[(gname + "T", i)])
                elif gname == "v":
                    S.add("act", lambda e, i=i, pp=pp: e.activation(
                        out=Vt[:, i, :, 0:64], in_=PS[pp][:].rearrange("p (h d) -> p h d", h=8), func=AF.Copy),
                        reads=[("ps", pp)], writes=[("Vt", i)])
                else:
                    S.add("act", lambda e, pp=pp: e.activation(out=iraw[:], in_=PS[pp][:, 0:324], func=AF.Copy),
                          reads=[("ps", pp)], writes=["iraw"])
                    qi3 = iraw[:, 0:256].rearrange("p (h d) -> p h d", h=4)
                    qib = qib_l[i % 2]
                    ki2b = ki2b_l[i % 2]
                    QIB = ("qib", i % 2)
                    KI2B = ("ki2b", i % 2)
                    qib3 = qib.rearrange("p (h d) -> p h d", h=4)
                    cosb = cst[:, O_COSI + i * 16:O_COSI + (i + 1) * 16].unsqueeze(1).to_broadcast([128, 4, 16])
                    sinb = cst[:, O_SINI + i * 16:O_SINI + (i + 1) * 16].unsqueeze(1).to_broadcast([128, 4, 16])
                    i3 = [t.rearrange("p (h d) -> p h d", h=4) for t in it]
                    x1, x2 = qi3[:, :, 0:16], qi3[:, :, 16:32]
                    S.add("dve", lambda e, x1=x1, cosb=cosb, i3=i3: e.tensor_tensor(out=i3[0], in0=x1, in1=cosb, op=ALU.mult),
                          reads=["iraw", "cst"], writes=[("it", 0)])
                    S.add("dve", lambda e, x2=x2, sinb=sinb, i3=i3: e.tensor_tensor(out=i3[1], in0=x2, in1=sinb, op=ALU.mult),
                          reads=["iraw", "cst"], writes=[("it", 1)])
                    S.add("dve", lambda e, i3=i3, qib3=qib3: e.tensor_tensor(out=qib3[:, :, 0:16], in0=i3[0], in1=i3[1], op=ALU.subtract),
                          reads=[("it", 0), ("it", 1)], writes=[QIB])
                    S.add("dve", lambda e, x2=x2, cosb=cosb, i3=i3: e.tensor_tensor(out=i3[2], in0=x2, in1=cosb, op=ALU.mult),
                          reads=["iraw", "cst"], writes=[("it", 2)])
                    S.add("dve", lambda e, x1=x1, sinb=sinb, i3=i3: e.tensor_tensor(out=i3[3], in0=x1, in1=sinb, op=ALU.mult),
                          reads=["iraw", "cst"], writes=[("it", 3)])
                    S.add("dve", lambda e, i3=i3, qib3=qib3: e.tensor_tensor(out=qib3[:, :, 16:32], in0=i3[2], in1=i3[3], op=ALU.add),
                          reads=[("it", 2), ("it", 3)], writes=[QIB])
                    S.add("dve", lambda e, qi3=qi3, qib3=qib3: e.tensor_copy(out=qib3[:, :, 32:64], in_=qi3[:, :, 32:64]),
                          reads=["iraw"], writes=[QIB])
                    kr = iraw[:, 256:320]
                    cos1 = cst[:, O_COSI + i * 16:O_COSI + (i + 1) * 16]
                    sin1 = cst[:, O_SINI + i * 16:O_SINI + (i + 1) * 16]
                    S.add("dve", lambda e, kr=kr, cos1=cos1: e.tensor_tensor(out=it[0][:, 0:16], in0=kr[:, 0:16], in1=cos1, op=ALU.mult),
                          reads=["iraw", "cst", QIB], writes=[("it", 0)])
                    S.add("dve", lambda e, kr=kr, sin1=sin1: e.tensor_tensor(out=it[1][:, 0:16], in0=kr[:, 16:32], in1=sin1, op=ALU.mult),
                          reads=["iraw", "cst", QIB], writes=[("it", 1)])
                    S.add("dve", lambda e, ki2b=ki2b: e.tensor_tensor(out=ki2b[:, 0:16], in0=it[0][:, 0:16], in1=it[1][:, 0:16], op=ALU.subtract),
                          reads=[("it", 0), ("it", 1)], writes=[KI2B])
                    S.add("dve", lambda e, kr=kr, cos1=cos1: e.tensor_tensor(out=it[2][:, 0:16], in0=kr[:, 16:32], in1=cos1, op=ALU.mult),
                          reads=["iraw", "cst", QIB], writes=[("it", 2)])
                    S.add("dve", lambda e, kr=kr, sin1=sin1: e.tensor_tensor(out=it[3][:, 0:16], in0=kr[:, 0:16], in1=sin1, op=ALU.mult),
                          reads=["iraw", "cst", QIB], writes=[("it", 3)])
                    S.add("dve", lambda e, ki2b=ki2b: e.tensor_tensor(out=ki2b[:, 16:32], in0=it[2][:, 0:16], in1=it[3][:, 0:16], op=ALU.add),
                          reads=[("it", 2), ("it", 3)], writes=[KI2B])
                    S.add("dve", lambda e, kr=kr, ki2b=ki2b: e.tensor_copy(out=ki2b[:, 32:64], in_=kr[:, 32:64]),
                          reads=["iraw"], writes=[KI2B])
                    S.add("dve", lambda e, ki2b=ki2b: e.tensor_copy(out=ki2b[:, 64:128], in_=ki2b[:, 0:64]),
                          reads=[KI2B], writes=[KI2B])
                    S.add("dve", lambda e, i=i: e.tensor_copy(out=wraw[:, i, :], in_=iraw[:, 320:324]),
                          reads=["iraw"], writes=[("wraw", i)])
                    pk = 6 + (i % 2)
                    for hp in range(2):
                        S.add("pe", lambda e, hp=hp, pk=pk, qib=qib: e.transpose(
                            out=psT_bf(pk)[:, hp * 128:(hp + 1) * 128], in_=qib[:, hp * 128:(hp + 1) * 128],
                            identity=identb[:, 0:128]),
                            reads=[QIB, "identb"], writes=[("ps", pk)])
                    S.add("pe", lambda e, pk=pk, ki2b=ki2b: e.transpose(
                        out=psT_bf(pk)[:, 256:384], in_=ki2b[:], identity=identb[:, 0:128]),
                        reads=[KI2B, "identb"], writes=[("ps", pk)])
                    S.add("act", lambda e, i=i, pk=pk: e.activation(
                        out=qiT[:, :, i * 128:(i + 1) * 128],
                        in_=psT_bf(pk)[:, 0:256].rearrange("p (h t) -> p h t", h=2), func=AF.Copy),
                        reads=[("ps", pk)], writes=[("qiT", i)])
                    S.add("act", lambda e, i=i, pk=pk: e.activation(
                        out=kiT2[:, i * 128:(i + 1) * 128], in_=psT_bf(pk)[:, 256:384], func=AF.Copy),
                        reads=[("ps", pk)], writes=[("kiT2", i)])
                tile_ops.append(S.end_capture())
            def _split(ops):
                for j, o in enumerate(ops):
                    if o[0] == 'pe' and any(w in (('ps', 6), ('ps', 7)) for w in o[3]):
                        return ops[:j], ops[j:]
                return ops, []
            parts = [_split(o) for o in tile_ops]
            for o in parts[0][0]:
                S.add(*o)
            for i in range(NT):
                if i + 1 < NT:
                    for o in parts[i + 1][0]:
                        S.add(*o)
                for o in parts[i][1]:
                    S.add(*o)
        wr2 = wraw[:].rearrange("p i h -> p (i h)")
        wa2 = wabs[:].rearrange("p i h -> p (i h)")
        ws2 = wsgn[:].rearrange("p i h -> p (i h)")
        allw = [("wraw", i) for i in range(NT)]
        S.add("dve", lambda e: e.tensor_scalar(out=wa2, in0=wr2, scalar1=-1.0, scalar2=None, op0=ALU.mult),
              reads=allw, writes=["wabs0"])
        S.add("dve", lambda e: e.tensor_tensor(out=wa2, in0=wa2, in1=wr2, op=ALU.max),
              reads=allw + ["wabs0"], writes=["wabs1"])
        S.add("dve", lambda e: e.tensor_scalar(out=wa2, in0=wa2, scalar1=1.0 / 16, scalar2=None, op0=ALU.mult),
              reads=["wabs1"], writes=["wabs"])
        S.add("dve", lambda e: e.tensor_scalar(out=ws2, in0=wr2, scalar1=0.0, scalar2=2.0, op0=ALU.is_ge, op1=ALU.mult),
              reads=allw, writes=["wsgn0"])
        S.add("dve", lambda e: e.tensor_scalar(out=ws2, in0=ws2, scalar1=-1.0, scalar2=None, op0=ALU.add),
              reads=["wsgn0"], writes=["wsgn"])

        dead = S.retire(lambda r: (isinstance(r, tuple) and r[0] in ("xnT", "xn_bf", "qraw", "rt", "qr", "it", "qib", "ki2b"))
                        or r in ("wst0", "wst1", "mixscr", "qsq", "qn", "iraw"))
        for nm in (["wouta", "attnT", "attn_tm"] + [("jm", a, b) for a in range(2) for b in range(2)]
                   + [("pT", i) for i in range(4)]
                   + [("acc", t, i) for t in range(2) for i in range(4)]):
            S.seed(nm, dead)
        QB = 256
        NQB = SEQ // QB
        accb = [view(RA, t * 8 * KB, 8 * KB, F32) for t in range(2)]
        junkD = view(RA, 16 * KB, 4 * KB, BF16)
        junkA = view(RA, 20 * KB, 4 * KB, BF16)
        jm4 = [view(RD, t * 4 * KB, 4 * KB, BF16) for t in range(4)]
        wouta = view(RD, 16 * KB, 8 * KB, BF16).rearrange("p (h d) -> p h d", h=4)
        attnT = view(RD, 24 * KB, 2 * KB, BF16).rearrange("p (h t) -> p h t", h=4)
        attn_tm = view(RD, 26 * KB, 2 * KB, BF16).rearrange("p (t c) -> p t c", t=2)
        pT = [view(RD, 28 * KB + i * KB, KB, BF16) for i in range(4)]
        S.add("pool", lambda e: e.dma_start(out=wouta[:], in_=wout_d[0:512, :].rearrange("(h p) d -> p h d", p=128)),
              writes=["wouta"], dma=True)
        trib = identb[:, 128:256]
        CAND, CNT, TT, MN, MX, TH = 0, 1, 2, 3, 4, 5
        pt_ctr = [0]

        def gen_A(qb):
            if qb == 0:
                j0, j1 = jm4[0], jm4[1]
                S.add("dve", lambda e: e.tensor_copy(out=j0[:, 0:128], in_=cbias), reads=["cst"], writes=[("jm", 0, 0)])
                S.add("dve", lambda e: e.memset(j1[:, 0:128], 0.0), writes=[("jm", 0, 1)])
                S.add("dve", lambda e: e.tensor_copy(out=j1[:, 128:256], in_=cbias), reads=["cst"], writes=[("jm", 0, 1)])
                return
            def tile_vars(tl):
                tq = 2 * qb + tl
                n = (tq + 1) * 128
                nchunk = (n + 511) // 512
                return tq, n, nchunk, accb[tl], jm4[(qb % 2) * 2 + tl], bis[:, tl * 8:(tl + 1) * 8], "b%d" % tl

            def stage_scores(tl):
                tq, n, nchunk, acc, jm, bb, bn = tile_vars(tl)
                for h in range(4):
                    hp, par = h // 2, h % 2
                    for sc in range(nchunk):
                        w = min(512, n - sc * 512)
                        pi = (h * nchunk + sc) % 2
                        S.add("pe", lambda e, hp=hp, par=par, sc=sc, w=w, pi=pi, tq=tq: e.matmul(
                            PS[pi][:, 0:w], lhsT=qiT[par * 64:(par + 1) * 64, hp, tq * 128:(tq + 1) * 128],
                            rhs=kiT2[par * 64:(par + 1) * 64, sc * 512:sc * 512 + w], start=True, stop=True),
                            reads=[("qiT", tq)] + [("kiT2", t) for t in range(sc * 4, sc * 4 + (w + 127) // 128)],
                            writes=[("ps", pi)])
                        S.add("act", lambda e, w=w, pi=pi, tq=tq, h=h: e.activation(
                            out=PS[pi][:, 0:w], in_=PS[pi][:, 0:w], func=AF.Relu, scale=wabs[:, tq, h:h + 1]),
                            reads=[("ps", pi), "wabs"], writes=[("ps", pi)])
                        if h == 0:
                            S.add("dve", lambda e, sc=sc, w=w, tq=tq, pi=pi, acc=acc: e.tensor_scalar(
                                out=acc[:, sc * 512:sc * 512 + w], in0=PS[pi][:, 0:w],
                                scalar1=wsgn[:, tq, 0:1], scalar2=None, op0=ALU.mult),
                                reads=[("ps", pi), "wsgn"], writes=[("acc", tl, sc)])
                        else:
                            S.add("dve", lambda e, sc=sc, w=w, tq=tq, h=h, pi=pi, acc=acc: e.scalar_tensor_tensor(
                                out=acc[:, sc * 512:sc * 512 + w], in0=PS[pi][:, 0:w],
                                scalar=wsgn[:, tq, h:h + 1], in1=acc[:, sc * 512:sc * 512 + w],
                                op0=ALU.mult, op1=ALU.add),
                                reads=[("ps", pi), "wsgn", ("acc", tl, sc)], writes=[("acc", tl, sc)])

            def stage_setup(tl):
                tq, n, nchunk, acc, jm, bb, bn = tile_vars(tl)
                accs = [("acc", tl, sc) for sc in range(nchunk)]
                S.add("dve", lambda e, n=n, acc=acc, bb=bb: e.tensor_reduce(out=bb[:, MN:MN + 1], in_=acc[:, 0:n], axis=AX.X, op=ALU.min),
                      reads=accs, writes=[bn + "mn"])
                S.add("dve", lambda e, tq=tq, acc=acc: e.tensor_tensor(out=acc[:, tq * 128:(tq + 1) * 128],
                                                                      in0=acc[:, tq * 128:(tq + 1) * 128], in1=cbias, op=ALU.add),
                      reads=accs + ["cst", bn + "mn"], writes=accs)
                S.add("dve", lambda e, n=n, acc=acc, bb=bb: e.tensor_reduce(out=bb[:, MX:MX + 1], in_=acc[:, 0:n], axis=AX.X, op=ALU.max),
                      reads=accs, writes=[bn + "mx"])
                S.add("dve", lambda e, bb=bb: e.tensor_tensor(out=bb[:, MX:MX + 1], in0=bb[:, MX:MX + 1], in1=bb[:, MN:MN + 1],
                                                              op=ALU.subtract), reads=[bn + "mx", bn + "mn"], writes=[bn + "rng"])
                S.add("dve", lambda e, bb=bb: e.tensor_scalar(out=bb[:, MX:MX + 1], in0=bb[:, MX:MX + 1], scalar1=1e-6,
                                                              scalar2=None, op0=ALU.max), reads=[bn + "rng"], writes=[bn + "rng2"])
                S.add("dve", lambda e, bb=bb: e.reciprocal(out=bb[:, MX:MX + 1], in_=bb[:, MX:MX + 1]),
                      reads=[bn + "rng2"], writes=[bn + "irng"])
                S.add("dve", lambda e, n=n, acc=acc, bb=bb: e.tensor_scalar(
                    out=acc[:, 0:n], in0=acc[:, 0:n], scalar1=bb[:, MN:MN + 1], scalar2=bb[:, MX:MX + 1],
                    op0=ALU.subtract, op1=ALU.mult),
                    reads=accs + [bn + "mn", bn + "irng"], writes=accs)
                S.add("dve", lambda e, bb=bb, tl=tl: e.memset(bb[:, CAND:CAND + 1], 0.5 if tl == 0 else -0.5),
                      writes=[bn + "cand"])

            def stage_iter(tl, it_):
                tq, n, nchunk, acc, jm, bb, bn = tile_vars(tl)
                accs = [("acc", tl, sc) for sc in range(nchunk)]
                if tl == 0:
                    S.add("dve", lambda e, n=n, acc=acc, bb=bb: e.tensor_scalar(
                        out=junkD[:, 0:n], in0=acc[:, 0:n], scalar1=bb[:, CAND:CAND + 1], scalar2=None,
                        op0=ALU.is_ge, op1=ALU.add, accum_out=bb[:, CNT:CNT + 1]),
                        reads=accs + [bn + "cand"], writes=[bn + "cnt"])
                    S.add("dve", lambda e, it_=it_, bb=bb: e.tensor_scalar(
                        out=bb[:, TT:TT + 1], in0=bb[:, CNT:CNT + 1], scalar1=TOPK - 0.5, scalar2=2.0 ** (-it_),
                        op0=ALU.is_ge, op1=ALU.mult), reads=[bn + "cnt"], writes=[bn + "tt"])
                    S.add("dve", lambda e, it_=it_, bb=bb: e.scalar_tensor_tensor(
                        out=bb[:, CAND:CAND + 1], in0=bb[:, TT:TT + 1], scalar=-(2.0 ** (-(it_ + 1))),
                        in1=bb[:, CAND:CAND + 1], op0=ALU.add, op1=ALU.add),
                        reads=[bn + "tt", bn + "cand"], writes=[bn + "cand"])
                else:
                    S.add("act", lambda e, n=n, acc=acc, bb=bb: e.activation(
                        out=junkA[:, 0:n], in_=acc[:, 0:n], func=AF.Sign, bias=bb[:, CAND:CAND + 1], scale=1.0,
                        accum_out=bb[:, CNT:CNT + 1]),
                        reads=accs + [bn + "cand"], writes=[bn + "cnt"])
                    S.add("act", lambda e, n=n, bb=bb: e.activation(
                        out=bb[:, TT:TT + 1], in_=bb[:, CNT:CNT + 1], func=AF.Sign, bias=float(n - 2 * TOPK + 1), scale=1.0),
                        reads=[bn + "cnt"], writes=[bn + "tt"])
                    S.add("act", lambda e, it_=it_, bb=bb: e.activation(
                        out=bb[:, CAND:CAND + 1], in_=bb[:, TT:TT + 1], func=AF.Identity,
                        bias=bb[:, CAND:CAND + 1], scale=-(2.0 ** (-(it_ + 1)))),
                        reads=[bn + "tt", bn + "cand"], writes=[bn + "cand"])

            def stage_mask(tl):
                tq, n, nchunk, acc, jm, bb, bn = tile_vars(tl)
                accs = [("acc", tl, sc) for sc in range(nchunk)]
                if tl == 0:
                    S.add("dve", lambda e, bb=bb: e.tensor_scalar(out=bb[:, TH:TH + 1], in0=bb[:, CAND:CAND + 1],
                                                                  scalar1=-(2.0 ** (-(NBIS + 1))), scalar2=None, op0=ALU.add),
                          reads=[bn + "cand"], writes=[bn + "th"])
                else:
                    S.add("dve", lambda e, bb=bb: e.tensor_scalar(out=bb[:, TH:TH + 1], in0=bb[:, CAND:CAND + 1],
                                                                  scalar1=-1.0, scalar2=-(2.0 ** (-(NBIS + 1))),
                                                                  op0=ALU.mult, op1=ALU.add),
                          reads=[bn + "cand"], writes=[bn + "th"])
                S.add("dve", lambda e, n=n, acc=acc, bb=bb, jm=jm: e.tensor_scalar(
                    out=jm[:, 0:n], in0=acc[:, 0:n], scalar1=bb[:, TH:TH + 1], scalar2=bis[:, 23:24],
                    op0=ALU.is_lt, op1=ALU.mult),
                    reads=accs + [bn + "th", "negbig"], writes=[("jm", qb % 2, tl)])

            for tl in range(2):
                stage_scores(tl)
                stage_setup(tl)
            for it_ in range(1, NBIS + 1):
                stage_iter(0, it_)
                stage_iter(1, it_)
            for tl in range(2):
                stage_mask(tl)

        def gen_B(qb):
            npair = qb + 1
            units = [(h, p) for h in range(8) for p in range(npair)]

            def front(k):
                h, p = units[k]
                hp, par = h // 2, h % 2
                pS = 2 + k % 2
                pti = k % 4
                for j in range(2):
                    sbk = 2 * p + j
                    c0 = 128 if sbk == 2 * qb + 1 else 0
                    S.add("pe", lambda e, hp=hp, par=par, sbk=sbk, c0=c0, pS=pS, j=j: e.matmul(
                        PS[pS][:, j * QB + c0:(j + 1) * QB], lhsT=kT[par * 64:(par + 1) * 64, hp, sbk * 128:(sbk + 1) * 128],
                        rhs=qT[par * 64:(par + 1) * 64, hp, qb * QB + c0:(qb + 1) * QB], start=(j == 0), stop=False,
                        skip_group_check=True),
                        reads=[("kT", sbk)] + [("qT", 2 * qb + t) for t in range(2)], writes=[("ps", pS)])
                    for tl in range(c0 // 128, 2):
                        jm = jm4[(qb % 2) * 2 + tl]
                        S.add("pe", lambda e, sbk=sbk, tl=tl, pS=pS, jm=jm, j=j: e.matmul(
                            PS[pS][:, j * QB + tl * 128:j * QB + (tl + 1) * 128], lhsT=jm[:, sbk * 128:(sbk + 1) * 128],
                            rhs=identb[:, 0:128], start=False, stop=(j == 1 and tl == 1), skip_group_check=True),
                            reads=[("jm", qb % 2, tl), "identb"], writes=[("ps", pS)])
                S.add("act", lambda e, pS=pS, pti=pti: e.activation(
                    out=pT[pti][:], in_=PS[pS][:], func=AF.Exp, scale=0.125),
                    reads=[("ps", pS)], writes=[("pT", pti)])

            def back(k):
                h, p = units[k]
                po = 4 + h % 2
                pti = k % 4
                for j in range(2):
                    sbk = 2 * p + j
                    c0 = 128 if sbk == 2 * qb + 1 else 0
                    for tl in range(c0 // 128, 2):
                        first = (sbk == 0 and tl == 0)
                        last = (sbk == 2 * qb + tl)
                        S.add("pe", lambda e, sbk=sbk, h=h, tl=tl, po=po, pti=pti, first=first, last=last, j=j: e.matmul(
                            PS[po][:, tl * 65:(tl + 1) * 65], lhsT=pT[pti][:, j * QB + tl * 128:j * QB + (tl + 1) * 128],
                            rhs=Vt[:, sbk, h, :], start=first, stop=last, skip_group_check=True),
                            reads=[("Vt", sbk), "V", ("pT", pti)], writes=[("ps", po)])
                if p == npair - 1:
                    pv = PS[po][:, 0:130].rearrange("p (t c) -> p t c", c=65)
                    rd = bis[:, 16 + 2 * (h % 2):18 + 2 * (h % 2)]
                    S.add("dve", lambda e, pv=pv, rd=rd: e.reciprocal(out=rd, in_=pv[:, :, 64]),
                          reads=[("ps", po)], writes=[("rden", h % 2)])
                    S.add("dve", lambda e, h=h, pv=pv, rd=rd: e.tensor_tensor(
                        out=attn_tm[:, :, h * 64:(h + 1) * 64], in0=pv[:, :, 0:64],
                        in1=rd.unsqueeze(2).to_broadcast([128, 2, 64]), op=ALU.mult),
                        reads=[("ps", po), ("rden", h % 2)], writes=["attn_tm"])

            front(0)
            for k in range(len(units)):
                if k + 1 < len(units):
                    front(k + 1)
                back(k)
            for tl in range(2):
                i = 2 * qb + tl
                for hp in range(4):
                    S.add("pe", lambda e, tl=tl, hp=hp: e.transpose(
                        out=psT_bf(7)[:, hp * 128:(hp + 1) * 128], in_=attn_tm[:, tl, hp * 128:(hp + 1) * 128],
                        identity=identb[:, 0:128]),
                        reads=["attn_tm", "identb"], writes=[("ps", 7)])
                S.add("act", lambda e, tl=tl: e.activation(
                    out=attnT[:, :, tl * 128:(tl + 1) * 128],
                    in_=psT_bf(7)[:, 0:512].rearrange("p (h t) -> p h t", h=4), func=AF.Copy),
                    reads=[("ps", 7)], writes=["attnT"])
                for dh in range(2):
                    for hp in range(4):
                        S.add("pe", lambda e, tl=tl, dh=dh, hp=hp: e.matmul(
                            PS[7][:], lhsT=attnT[:, hp, tl * 128:(tl + 1) * 128],
                            rhs=wouta[:, hp, dh * 512:(dh + 1) * 512], start=(hp == 0), stop=(hp == 3)),
                            reads=["attnT", "wouta"], writes=[("ps", 7)])
                    S.add("dve", lambda e, i=i, dh=dh: e.tensor_tensor(
                        out=resid[:, i, dh * 512:(dh + 1) * 512], in0=PS[7][:], in1=resid[:, i, dh * 512:(dh + 1) * 512],
                        op=ALU.add),
                        reads=[("ps", 7), ("resid", i, dh)], writes=[("resid", i, dh)])

        gen_A(0)
        for qb in range(NQB):
            S.begin_capture()
            gen_B(qb)
            lb = S.end_capture()
            la = []
            if qb + 1 < NQB:
                S.begin_capture()
                gen_A(qb + 1)
                la = S.end_capture()
            S.merge([lb, la])

        dead = S.retire(lambda r: (isinstance(r, tuple) and r[0] in ("acc", "jm", "maskT", "pT", "qT", "kT", "Vt", "qiT", "kiT2"))
                        or r in ("attnT", "attn_tm", "wouta", "rden", "V"))
        for nm in ([("xnT", i) for i in range(NT)] + ["grep", ("xn_bf", 0), ("xn_bf", 1), ("wdb", 0), ("wdb", 1),
                   ("wgb", 0), ("wgb", 1), ("wub", 0), ("wub", 1), ("sgb", 0), ("sgb", 1)]
                   + [("hT", c, t) for c in range(6) for t in range(4)]):
            S.seed(nm, dead)
        ffn(1, 2, False, True)
        S.add("sp", lambda e: e.nop(), reads=[("out", i) for i in range(NT)])
        S.emit(st)
    return nc


def _consts():
    c = np.zeros((128, NCST), np.float32)
    p = np.arange(128)
    c[:, O_CB:O_CB + 128] = np.where(p[None, :] <= p[:, None], 0.0, NEG).astype(np.float32)
    pos = (np.arange(NT)[None, :] * 128 + p[:, None]).astype(np.float32)
    for half, oc, os_ in ((32, O_COSQ, O_SINQ), (16, O_COSI, O_SINI)):
        inv = (np.float32(10000.0) ** (-np.arange(half, dtype=np.float32) / np.float32(half))).astype(np.float32)
        ang = (pos[:, :, None] * inv[None, None, :]).astype(np.float32)
        c[:, oc:oc + NT * half] = np.cos(ang).astype(np.float32).reshape(128, -1)
        c[:, os_:os_ + NT * half] = np.sin(ang).astype(np.float32).reshape(128, -1)
    c[:, O_INVC:O_INVC + 16] = (1.0 / np.arange(1, 17, dtype=np.float32))[None, :]
    return c


_PROG = None


def kernel(x, ffn1_norm, ffn1_w_gate, ffn1_w_up, ffn1_w_down, mix_norm, w_in, q_norm, k_norm,
           pool_w, pool_scale, w_out, ffn2_norm, ffn2_w_gate, ffn2_w_up, ffn2_w_down):
    global _PROG
    f = lambda a: np.ascontiguousarray(np.asarray(a, dtype=np.float32))
    x = f(x)
    cst = _consts()
    cst[:, O_G1:O_G1 + 8] = f(ffn1_norm)[0].reshape(8, 128).T
    cst[:, O_GM:O_GM + 8] = f(mix_norm)[0].reshape(8, 128).T
    cst[:, O_G2:O_G2 + 8] = f(ffn2_norm)[0].reshape(8, 128).T
    cst[:, O_GQ:O_GQ + 64] = f(q_norm)[0][None, :]
    cst[:, O_GK:O_GK + 64] = f(k_norm)[0][None, :]
    cst[:, O_PS:O_PS + 4] = f(pool_scale)[0].reshape(4, 128).T
    shared = {
        "ffn1_w_gate": f(ffn1_w_gate)[0], "ffn2_w_gate": f(ffn2_w_gate)[0],
        "ffn1_w_up": f(ffn1_w_up)[0], "ffn2_w_up": f(ffn2_w_up)[0],
        "ffn1_w_down": f(ffn1_w_down)[0], "ffn2_w_down": f(ffn2_w_down)[0],
        "w_in": f(w_in)[0], "w_out": f(w_out)[0], "pool_w": f(pool_w)[0],
        "grep": np.ascontiguousarray(np.broadcast_to(
            np.stack([f(ffn1_norm)[0], f(mix_norm)[0], f(ffn2_norm)[0]])[:, None, :], (3, 128, DM))),
        "cst": cst, "ident": np.concatenate([np.eye(128, dtype=np.float32), np.triu(np.ones((128, 128), np.float32))], axis=1),
    }
    if _PROG is None:
        _PROG = build_program()
    n = x.shape[0]
    in_maps = [dict(shared, x=x[b]) for b in range(n)]
    res = run_bass_kernel_spmd(_PROG, in_maps, core_ids=list(range(n)))
    return np.stack([np.asarray(r["out"], dtype=np.float32) for r in res.results], axis=0)
```

```python
import math
from contextlib import ExitStack
import numpy as np
import concourse.bass as bass
import concourse.mybir as mybir
from concourse.bass_utils import run_bass_kernel_spmd

F32 = mybir.dt.float32
BF16 = mybir.dt.bfloat16
AF = mybir.ActivationFunctionType
ALU = mybir.AluOpType
AX = mybir.AxisListType

SEQ = 2048
DM = 1024
DFF = 2816
NT = SEQ // 128
NCH = DFF // 128
EPS = 1e-6
TOPK = 256
NBIS = 16
NEG = -1.0e30
CQ, CK, CV, CQI, CKI, CWI, CU = 0, 512, 1024, 1536, 1792, 1856, 1860
O_CB, O_COSQ, O_SINQ, O_COSI, O_SINI, O_INVC = 0, 128, 640, 1152, 1408, 1664
O_G1, O_GM, O_G2, O_GQ, O_GK, O_PS = 1680, 1688, 1696, 1704, 1768, 1832
NCST = 1836


class _Op:
    __slots__ = ("eng", "fn", "deps", "idx", "is_dma", "sem", "val", "milestone")


class Sched:
    ENG = ("pe", "act", "dve", "pool", "sp")
    ROT = 900

    def __init__(self, nc, n_dma_sems=20):
        self.nc = nc
        self.ops = []
        self.last_w = {}
        self.readers = {}
        self.n_dma_sems = n_dma_sems
        self.cap = None
        self.cap_stack = []

    def add(self, eng, fn, reads=(), writes=(), dma=False):
        if self.cap is not None:
            self.cap.append((eng, fn, tuple(reads), tuple(writes), dma))
            return None
        op = _Op()
        op.eng = eng; op.fn = fn; op.is_dma = dma; op.idx = len(self.ops)
        op.milestone = False; op.sem = None; op.val = 0
        deps = set()
        for r in reads:
            w = self.last_w.get(r)
            if w is not None:
                deps.add(w)
        for r in writes:
            w = self.last_w.get(r)
            if w is not None:
                deps.add(w)
            for rd in self.readers.get(r, ()):
                deps.add(rd)
        op.deps = sorted(deps)
        for r in reads:
            self.readers.setdefault(r, []).append(op.idx)
        for r in writes:
            self.last_w[r] = op.idx
            self.readers[r] = []
        self.ops.append(op)
        return op.idx

    def begin_capture(self):
        self.cap_stack.append(self.cap)
        self.cap = []

    def end_capture(self):
        c = self.cap
        self.cap = self.cap_stack.pop()
        return c

    def merge(self, lists):
        lists = [l for l in lists if l]
        pos = [0] * len(lists)
        while True:
            best, bf = None, None
            for k, l in enumerate(lists):
                if pos[k] < len(l):
                    f = pos[k] / len(l)
                    if bf is None or f < bf:
                        best, bf = k, f
            if best is None:
                break
            self.add(*lists[best][pos[best]])
            pos[best] += 1

    def retire(self, pred):
        out = set()
        for r in list(self.last_w.keys()):
            if pred(r):
                out.add(self.last_w.pop(r))
        for r in list(self.readers.keys()):
            if pred(r):
                out.update(self.readers.pop(r))
        return out

    def seed(self, name, opset):
        self.readers.setdefault(name, []).extend(sorted(opset))

    def emit(self, stack):
        nc = self.nc
        ops = self.ops
        for op in ops:
            for d in op.deps:
                p = ops[d]
                if p.eng == "pe" and op.eng == "pe" and not p.is_dma and not op.is_dma:
                    continue
                p.milestone = True
        n_ms = {e: 0 for e in self.ENG}
        for op in ops:
            if op.milestone and not op.is_dma:
                n_ms[op.eng] += 1
        esems = {e: [stack.enter_context(nc.semaphore("s_%s%d" % (e, i)))
                     for i in range(n_ms[e] // self.ROT + 1)] for e in self.ENG}
        dsems = [stack.enter_context(nc.semaphore("d%d" % i)) for i in range(self.n_dma_sems)]
        ecount = {e: 0 for e in self.ENG}
        dcount = [0] * self.n_dma_sems
        dlast = [None] * self.n_dma_sems
        dnext = {e: 0 for e in self.ENG}
        prev_on_dsem = {}
        for op in ops:
            if op.is_dma:
                half = self.n_dma_sems // 2
                base = 0 if op.eng == "pool" else half
                j = base + dnext[op.eng] % half
                dnext[op.eng] += 1
                if dlast[j] is not None:
                    prev_on_dsem[op.idx] = dlast[j]
                dcount[j] += 16
                op.sem = dsems[j]; op.val = dcount[j]; dlast[j] = op.idx
                op.milestone = True
            elif op.milestone:
                c = ecount[op.eng]
                ecount[op.eng] += 1
                op.sem = esems[op.eng][c // self.ROT]; op.val = c % self.ROT + 1
        per_eng = {e: [o for o in ops if o.eng == e] for e in self.ENG}
        block = stack.enter_context(nc.Block())

        def run(engname, eng):
            waited = {}
            for op in per_eng[engname]:
                deps = list(op.deps)
                if op.idx in prev_on_dsem:
                    deps.append(prev_on_dsem[op.idx])
                need = {}
                for d in deps:
                    p = ops[d]
                    if p.eng == "pe" and op.eng == "pe" and not p.is_dma and not op.is_dma:
                        continue
                    key = id(p.sem)
                    if waited.get(key, 0) >= p.val:
                        continue
                    if key not in need or need[key][1] < p.val:
                        need[key] = (p.sem, p.val)
                for key, (sem, val) in need.items():
                    eng.wait_ge(sem, val)
                    waited[key] = val
                ins = op.fn(eng)
                if op.is_dma:
                    ins.then_inc(op.sem, 16)
                elif op.milestone:
                    ins.then_inc(op.sem, 1)

        @block.tensor
        def _(e):
            run("pe", e)

        @block.scalar
        def _(e):
            run("act", e)

        @block.vector
        def _(e):
            run("dve", e)

        @block.gpsimd
        def _(e):
            run("pool", e)

        @block.sync
        def _(e):
            run("sp", e)


def build_program():
    nc = bass.Bass("TRN2", target_bir_lowering=False)

    def din(name, shape):
        return nc.dram_tensor(name, shape, F32, kind="ExternalInput").ap()

    x_d = din("x", [SEQ, DM])
    wg_d = [din("ffn1_w_gate", [DM, DFF]), din("ffn2_w_gate", [DM, DFF])]
    wu_d = [din("ffn1_w_up", [DM, DFF]), din("ffn2_w_up", [DM, DFF])]
    wd_d = [din("ffn1_w_down", [DFF, DM]), din("ffn2_w_down", [DFF, DM])]
    win_d = din("w_in", [DM, 2372])
    wout_d = din("w_out", [DM, DM])
    poolw_d = din("pool_w", [4, 128, 128])
    cst_d = din("cst", [128, NCST])
    ident_d = din("ident", [128, 256])
    grep_d = din("grep", [3, 128, DM])
    out_d = nc.dram_tensor("out", [SEQ, DM], F32, kind="ExternalOutput").ap()

    with ExitStack() as st:
        def sb(name, shape, dt=F32):
            return st.enter_context(nc.sbuf_tensor(name, shape, dt))

        resid = sb("resid", [128, NT, DM], F32)
        RA = sb("RA", [128, 16384], BF16)
        RC = sb("RC", [128, 31232], BF16)
        RD = sb("RD", [128, 20480], BF16)
        cst = sb("cst_sb", [128, NCST], F32)
        identb = sb("identb", [128, 256], BF16)
        ones = sb("ones", [128, 64], F32)
        ss = sb("ss", [128, NT], F32)
        rstd = sb("rstd", [128, NT], F32)
        ss8 = sb("ss8", [128, 2, 8], F32)
        rs8 = sb("rs8", [128, 2, 8], F32)
        wraw = sb("wraw", [128, NT, 4], F32)
        wabs = sb("wabs", [128, NT, 4], F32)
        wsgn = sb("wsgn", [128, NT, 4], F32)
        bis = sb("bis", [128, 24], F32)
        PS = [st.enter_context(nc.psum_tensor("ps%d" % i, [128, 512], F32)) for i in range(8)]

        def view(reg, off_b, nbytes, dt):
            v = reg[:, off_b // 2:(off_b + nbytes) // 2]
            return v if dt == BF16 else v.bitcast(dt)

        KB = 1024
        S = Sched(nc)

        S.add("sp", lambda e: e.dma_start(out=cst[:], in_=cst_d[:]), writes=["cst"], dma=True)
        S.add("pool", lambda e: e.dma_start(out=identb[:], in_=ident_d[:]), writes=["identb"], dma=True)
        S.add("dve", lambda e: e.memset(ones[:], 1.0), writes=["ones"])
        S.add("dve", lambda e: e.memset(bis[:, 23:24], -30000.0), writes=["negbig"])
        cbias = cst[:, O_CB:O_CB + 128]

        def gT(off):
            return cst[:, off:off + 8]

        def psT_bf(k):
            return PS[k][:].bitcast(BF16)

        xn_bf = [view(RD, 16 * KB + i * 2 * KB, 2 * KB, BF16) for i in range(2)]
        xnT = RA[:].rearrange("p (k t) -> p k t", k=8)
        g_rep = view(RD, 24 * KB, 4 * KB, F32)

        def rms_to_T(tag, gidx, load_x):
            S.add("sp", lambda e: e.dma_start(out=g_rep[:], in_=grep_d[gidx]), writes=["grep"], dma=True)
            S.add("dve", lambda e: e.memset(ss[:], 0.0), writes=["ss"])
            for i in range(NT):
                if load_x:
                    S.add("sp", lambda e, i=i: e.dma_start(out=resid[:, i, :], in_=x_d[i * 128:(i + 1) * 128, :]),
                          writes=[("resid", i, 0), ("resid", i, 1)], dma=True)
                S.add("act", lambda e, i=i: e.activation(out=xn_bf[i % 2][:], in_=resid[:, i, :], func=AF.Square,
                                                         accum_out=ss[:, i:i + 1]),
                      reads=[("resid", i, 0), ("resid", i, 1), "ss"], writes=[("xn_bf", i % 2), ("ssv", i)])
            S.add("act", lambda e: e.activation(out=rstd[:], in_=ss[:], func=AF.Sqrt, scale=1.0 / DM, bias=EPS),
                  reads=[("ssv", i) for i in range(NT)] + ["ss"], writes=["rstd0"])
            S.add("dve", lambda e: e.reciprocal(out=rstd[:], in_=rstd[:]), reads=["rstd0"], writes=["rstd"])
            for i in range(NT):
                S.add("dve", lambda e, i=i: e.scalar_tensor_tensor(
                    out=xn_bf[i % 2][:], in0=resid[:, i, :], scalar=rstd[:, i:i + 1], in1=g_rep[:],
                    op0=ALU.mult, op1=ALU.mult),
                    reads=[("resid", i, 0), ("resid", i, 1), "rstd", "grep"], writes=[("xn_bf", i % 2)])
                pk = 6 + (i % 2)
                for k in range(8):
                    S.add("pe", lambda e, i=i, k=k, pk=pk: e.transpose(
                        out=psT_bf(pk)[:, k * 128:(k + 1) * 128], in_=xn_bf[i % 2][:, k * 128:(k + 1) * 128],
                        identity=identb[:, 0:128]),
                        reads=[("xn_bf", i % 2), "identb"], writes=[("ps", pk)])
                S.add("act", lambda e, i=i, pk=pk: e.activation(
                    out=xnT[:, :, i * 128:(i + 1) * 128],
                    in_=psT_bf(pk).rearrange("p (k t) -> p k t", k=8), func=AF.Copy),
                    reads=[("ps", pk)], writes=[("xnT", i)])

        GROUPS = [(0, 6), (6, 6), (12, 6), (18, 4)]
        hT = view(RC, 0, 24 * KB, BF16).rearrange("p (c t) -> p c t", c=6)
        wdb = [view(RC, 24 * KB + i * 12 * KB, 12 * KB, BF16).rearrange("p (c d) -> p c d", c=6) for i in range(2)]
        wgb = [view(RD, i * 4 * KB, 4 * KB, BF16).rearrange("p (k f) -> p k f", k=8) for i in range(2)]
        wub = [view(RD, 8 * KB + i * 4 * KB, 4 * KB, BF16).rearrange("p (k f) -> p k f", k=8) for i in range(2)]
        sgb = [view(RD, 20 * KB + i * 2 * KB, 2 * KB, F32) for i in range(2)]

        def ffn(li, goff, load_x, final):
            rms_to_T("f%d" % li, goff, load_x)
            pair_ctr = [0]
            for gi, (c0, ncg) in enumerate(GROUPS):
                wd_t = wdb[gi % 2]

                def _wd_dma(c0=c0, ncg=ncg, wd_t=wd_t, gi=gi):
                    S.add("pool", lambda e: e.dma_start(
                        out=wd_t[:, 0:ncg, :],
                        in_=wd_d[li][c0 * 128:(c0 + ncg) * 128, :].rearrange("(c p) d -> p c d", p=128)),
                        writes=[("wdb", gi % 2)], dma=True)
                for pr in range(ncg // 2):
                    j = pair_ctr[0] % 2
                    pair_ctr[0] += 1
                    col0 = (c0 + 2 * pr) * 128
                    S.add("pool", lambda e, j=j, col0=col0: e.dma_start(
                        out=wgb[j][:], in_=wg_d[li][:, col0:col0 + 256].rearrange("(k p) f -> p k f", p=128)),
                        writes=[("wgb", j)], dma=True)
                    S.add("pool", lambda e, j=j, col0=col0: e.dma_start(
                        out=wub[j][:], in_=wu_d[li][:, col0:col0 + 256].rearrange("(k p) f -> p k f", p=128)),
                        writes=[("wub", j)], dma=True)
                    if pr == 0:
                        _wd_dma()
                    for cc in range(2):
                        cl = 2 * pr + cc
                        for tb in range(4):
                            q = (cl * 4 + tb) % 2
                            pg, pu = q, 2 + q
                            for k in range(8):
                                S.add("pe", lambda e, j=j, cc=cc, tb=tb, k=k, pg=pg: e.matmul(
                                    PS[pg][:], lhsT=wgb[j][:, k, cc * 128:(cc + 1) * 128],
                                    rhs=xnT[:, k, tb * 512:(tb + 1) * 512], start=(k == 0), stop=(k == 7)),
                                    reads=[("wgb", j)] + [("xnT", 4 * tb + t) for t in range(4)], writes=[("ps", pg)])
                            for k in range(8):
                                S.add("pe", lambda e, j=j, cc=cc, tb=tb, k=k, pu=pu: e.matmul(
                                    PS[pu][:], lhsT=wub[j][:, k, cc * 128:(cc + 1) * 128],
                                    rhs=xnT[:, k, tb * 512:(tb + 1) * 512], start=(k == 0), stop=(k == 7)),
                                    reads=[("wub", j)] + [("xnT", 4 * tb + t) for t in range(4)], writes=[("ps", pu)])
                            S.add("act", lambda e, q=q, pg=pg: e.activation(out=sgb[q][:], in_=PS[pg][:], func=AF.Silu),
                                  reads=[("ps", pg)], writes=[("sgb", q)])
                            S.add("dve", lambda e, q=q, pu=pu, cl=cl, tb=tb: e.tensor_tensor(
                                out=hT[:, cl, tb * 512:(tb + 1) * 512], in0=sgb[q][:], in1=PS[pu][:], op=ALU.mult),
                                reads=[("sgb", q), ("ps", pu)], writes=[("hT", cl, tb)])
                for i in range(NT):
                    for dh in range(2):
                        pd = 4 + (i * 2 + dh) % 2
                        for cl in range(ncg):
                            S.add("pe", lambda e, i=i, dh=dh, cl=cl, pd=pd, wd_t=wd_t, ncg=ncg: e.matmul(
                                PS[pd][:], lhsT=hT[:, cl, i * 128:(i + 1) * 128],
                                rhs=wd_t[:, cl, dh * 512:(dh + 1) * 512], start=(cl == 0), stop=(cl == ncg - 1)),
                                reads=[("hT", cl, i // 4), ("wdb", gi % 2)], writes=[("ps", pd)])
                        S.add("dve", lambda e, i=i, dh=dh, pd=pd: e.scalar_tensor_tensor(
                            out=resid[:, i, dh * 512:(dh + 1) * 512], in0=PS[pd][:], scalar=0.5,
                            in1=resid[:, i, dh * 512:(dh + 1) * 512], op0=ALU.mult, op1=ALU.add),
                            reads=[("ps", pd), ("resid", i, dh)], writes=[("resid", i, dh)])
                    if final and gi == len(GROUPS) - 1:
                        S.add("sp", lambda e, i=i: e.dma_start(out=out_d[i * 128:(i + 1) * 128, :], in_=resid[:, i, :]),
                              reads=[("resid", i, 0), ("resid", i, 1)], writes=[("out", i)], dma=True)

        ffn(0, 0, True, False)

        rms_to_T("mix", 1, False)
        dead = S.retire(lambda r: isinstance(r, tuple) and r[0] in ("hT", "wdb", "wgb", "wub", "sgb"))
        for nm in ["ubr", "wst0", "wst1", "mixscr"]:
            S.seed(nm, dead)
        dead_g = S.retire(lambda r: r == "grep")
        for nm in ["qsq", "qn", "grep"]:
            S.seed(nm, dead_g)

        UW = 16 + SEQ
        ubuf = [view(RC, i * 8256, 8256, F32) for i in range(4)]
        p2T = view(RC, 33024, 16 * KB, BF16).rearrange("p (g t) -> p g t", g=4)
        woutp = view(RC, 33024 + 16 * KB, 8 * KB, BF16).rearrange("p (g d) -> p g d", g=4)
        plT = view(RC, 33024 + 24 * KB, 4 * KB, BF16)
        poolw = view(RD, 38 * KB, 1 * KB, BF16).rearrange("p (g d) -> p g d", g=4)
        wst = [view(RD, i * 8 * KB, 8 * KB, BF16).rearrange("p (k f) -> p k f", k=8) for i in range(2)]
        UB = [("ubr", "u", 0), ("ubr", "u", 1), ("ubr", "s", 0), ("ubr", "s", 1)]
        for nm in UB + [("ubr", "plT"), ("ubr", "woutp"), ("ubr", "poolw")] + [("ubr", "p2T", g) for g in range(4)]:
            S.seed(nm, dead)
        S.add("pool", lambda e: e.dma_start(out=wst[0][:], in_=win_d[:, CU:CU + 512].rearrange("(k p) f -> p k f", p=128)),
              writes=["wst0"], dma=True)
        S.add("pool", lambda e: e.dma_start(out=woutp[:], in_=wout_d[512:1024, :].rearrange("(g p) d -> p g d", p=128)),
              writes=[("ubr", "woutp")], dma=True)
        S.add("pool", lambda e: e.dma_start(out=poolw[:], in_=poolw_d.rearrange("g c d -> c g d")),
              writes=[("ubr", "poolw")], dma=True)
        for b in range(4):
            S.add("dve", lambda e, b=b: e.memset(ubuf[b][:, 0:16], 0.0), writes=[UB[b]])

        def emit_U(g):
            u = ubuf[g % 2]
            for tb in range(4):
                pp = tb % 2
                for k in range(8):
                    S.add("pe", lambda e, g=g, tb=tb, k=k, pp=pp: e.matmul(
                        PS[pp][:], lhsT=wst[0][:, k, g * 128:(g + 1) * 128], rhs=xnT[:, k, tb * 512:(tb + 1) * 512],
                        start=(k == 0), stop=(k == 7)),
                        reads=["wst0"] + [("xnT", 4 * tb + t) for t in range(4)], writes=[("ps", pp)])
                S.add("act", lambda e, tb=tb, pp=pp, u=u: e.activation(out=u[:, 16 + tb * 512:16 + (tb + 1) * 512],
                                                                        in_=PS[pp][:], func=AF.Copy),
                      reads=[("ps", pp)], writes=[UB[g % 2]])

        def emit_pool(g):
            u = ubuf[g % 2]
            ures = UB[g % 2]
            nxt = [ubuf[2], ubuf[3]]
            nres = [UB[2], UB[3]]
            cur, cres = u, ures
            for stp in range(g + 1):
                sh = 1 << stp
                dst, dres = nxt[stp % 2], nres[stp % 2]
                S.add("dve", lambda e, cur=cur, dst=dst, sh=sh: e.tensor_tensor(
                    out=dst[:, 16:UW], in0=cur[:, 16:UW], in1=cur[:, 16 - sh:UW - sh], op=ALU.add),
                    reads=[cres], writes=[dres])
                cur, cres = dst, dres
            w = 2 << g
            S.add("dve", lambda e, cur=cur, u=u, w=w: e.scalar_tensor_tensor(
                out=plT[:], in0=cur[:, 16:UW], scalar=1.0 / w, in1=u[:, 16:UW], op0=ALU.mult, op1=ALU.subtract),
                reads=[cres, ures], writes=[("ubr", "plT")])
            tmpc, tres = nxt[(g + 1) % 2], nres[(g + 1) % 2]
            S.add("dve", lambda e, cur=cur, tmpc=tmpc, w=w: e.tensor_tensor(
                out=tmpc[:, 16:16 + w - 1], in0=cur[:, 16:16 + w - 1], in1=cst[:, O_INVC:O_INVC + w - 1], op=ALU.mult),
                reads=[cres, "cst"], writes=[tres])
            S.add("dve", lambda e, tmpc=tmpc, u=u, w=w: e.tensor_tensor(
                out=plT[:, 0:w - 1], in0=tmpc[:, 16:16 + w - 1], in1=u[:, 16:16 + w - 1], op=ALU.subtract),
                reads=[tres, ures], writes=[("ubr", "plT")])

        def emit_PM(g):
            for tb in range(4):
                pp = 2 + tb % 2
                S.add("pe", lambda e, g=g, tb=tb, pp=pp: e.matmul(
                    PS[pp][:], lhsT=poolw[:, g, :], rhs=plT[:, tb * 512:(tb + 1) * 512], start=True, stop=True),
                    reads=[("ubr", "plT"), ("ubr", "poolw")], writes=[("ps", pp)])
                S.add("act", lambda e, g=g, tb=tb, pp=pp: e.activation(
                    out=p2T[:, g, tb * 512:(tb + 1) * 512], in_=PS[pp][:], func=AF.Copy,
                    scale=cst[:, O_PS + g:O_PS + g + 1]),
                    reads=[("ps", pp), "cst"], writes=[("ubr", "p2T", g)])

        emit_U(0)
        for g in range(4):
            if g + 1 < 4:
                emit_U(g + 1)
            emit_pool(g)
            emit_PM(g)
        for i in range(NT):
            for dh in range(2):
                pd = 4 + (i * 2 + dh) % 2
                for g in range(4):
                    S.add("pe", lambda e, i=i, dh=dh, g=g, pd=pd: e.matmul(
                        PS[pd][:], lhsT=p2T[:, g, i * 128:(i + 1) * 128], rhs=woutp[:, g, dh * 512:(dh + 1) * 512],
                        start=(g == 0), stop=(g == 3)),
                        reads=[("ubr", "p2T", g), ("ubr", "woutp")], writes=[("ps", pd)])
                S.add("dve", lambda e, i=i, dh=dh, pd=pd: e.tensor_tensor(
                    out=resid[:, i, dh * 512:(dh + 1) * 512], in0=PS[pd][:], in1=resid[:, i, dh * 512:(dh + 1) * 512],
                    op=ALU.add),
                    reads=[("ps", pd), ("resid", i, dh)], writes=[("resid", i, dh)])
        dead = S.retire(lambda r: isinstance(r, tuple) and r[0] == "ubr")
        for nm in ["qT", "kT", "V", "qiT", "kiT2"]:
            S.seed(nm, dead)

        qT = view(RC, 0, 16 * KB, BF16).rearrange("p (h t) -> p h t", h=4)
        kT = view(RC, 16 * KB, 16 * KB, BF16).rearrange("p (h t) -> p h t", h=4)
        Vt = view(RC, 32 * KB, 16640, BF16).rearrange("p (i h d) -> p i h d", i=NT, h=8)
        qiT = view(RC, 32 * KB + 16640, 8 * KB, BF16).rearrange("p (h t) -> p h t", h=2)
        kiT2 = view(RC, 40 * KB + 16640, 4 * KB, BF16)
        MS = 20 * KB
        qraw = [view(RD, MS + i * 2 * KB, 2 * KB, F32) for i in range(2)]
        qsq = view(RD, MS + 4 * KB, 2 * KB, F32)
        qn = view(RD, MS + 6 * KB, 2 * KB, F32)
        rt = [view(RD, MS + 8 * KB + i * KB, KB, F32) for i in range(4)]
        qr = [view(RD, MS + 12 * KB + i * KB, KB, BF16) for i in range(2)]
        iraw = view(RD, MS + 14 * KB, 1296, F32)
        qib_l = [view(RD, MS + 14 * KB + 1296, 512, BF16), view(RD, MS + 14 * KB + 3088, 512, BF16)]
        ki2b_l = [view(RD, MS + 14 * KB + 1808, 256, BF16), view(RD, MS + 14 * KB + 3600, 256, BF16)]
        it = [view(RD, MS + 14 * KB + 2064 + i * 256, 256, F32) for i in range(4)]

        S.add("dve", lambda e: e.memset(Vt[:, :, :, 64:65], 1.0), writes=["V"])

        groups = [("q", CQ, 512), ("k", CK, 512), ("v", CV, 512), ("i", CQI, 324)]
        grp_lists = []
        for gi, (gname, coff, ncol) in enumerate(groups):
            wj = (gi + 1) % 2
            if gname in ("v", "i"):
                S.begin_capture()
            S.add("pool", lambda e, wj=wj, coff=coff, ncol=ncol: e.dma_start(
                out=wst[wj][:, :, 0:ncol], in_=win_d[:, coff:coff + ncol].rearrange("(k p) f -> p k f", p=128)),
                writes=["wst%d" % wj], dma=True)
            tile_ops = []
            for i in range(NT):
                S.begin_capture()
                pp = (2 if gname == "v" else 0) + i % 2
                for k in range(8):
                    S.add("pe", lambda e, i=i, k=k, pp=pp, wj=wj, ncol=ncol: e.matmul(
                        PS[pp][:, 0:ncol], lhsT=xnT[:, k, i * 128:(i + 1) * 128], rhs=wst[wj][:, k, 0:ncol],
                        start=(k == 0), stop=(k == 7)),
                        reads=["wst%d" % wj, ("xnT", i)], writes=[("ps", pp)])
                if gname in ("q", "k"):
                    goff = O_GQ if gname == "q" else O_GK
                    dstT = qT if gname == "q" else kT
                    qa = qraw[i % 2]
                    qrb = qr[i % 2]
                    q3 = qa.rearrange("p (h d) -> p h d", h=8)
                    S.add("act", lambda e, pp=pp, qa=qa: e.activation(out=qa[:], in_=PS[pp][:], func=AF.Copy),
                          reads=[("ps", pp)], writes=[("qraw", i % 2)])
                    S.add("dve", lambda e, qa=qa: e.tensor_tensor(out=qsq[:], in0=qa[:], in1=qa[:], op=ALU.mult),
                          reads=[("qraw", i % 2)], writes=["qsq"])
                    S.add("dve", lambda e, i=i: e.tensor_reduce(out=ss8[:, i % 2, :], in_=qsq.rearrange("p (h d) -> p h d", h=8),
                                                                axis=AX.X, op=ALU.add),
                          reads=["qsq"], writes=[("ss8", i % 2)])
                    S.add("act", lambda e, i=i: e.activation(out=rs8[:, i % 2, :], in_=ss8[:, i % 2, :], func=AF.Sqrt,
                                                             scale=1.0 / 64, bias=EPS),
                          reads=[("ss8", i % 2)], writes=[("rs8a", i % 2)])
                    qn3 = qn.rearrange("p (h d) -> p h d", h=8)
                    S.add("dve", lambda e, goff=goff, q3=q3: e.tensor_tensor(
                        out=qn3, in0=q3, in1=cst[:, goff:goff + 64].unsqueeze(1).to_broadcast([128, 8, 64]), op=ALU.mult),
                        reads=[("qraw", i % 2), "cst"], writes=["qn"])
                    S.add("dve", lambda e, i=i: e.reciprocal(out=rs8[:, i % 2, :], in_=rs8[:, i % 2, :]),
                          reads=[("rs8a", i % 2)], writes=[("rs8", i % 2)])
                    S.add("dve", lambda e, i=i: e.tensor_tensor(
                        out=qn3, in0=qn3, in1=rs8[:, i % 2, :].unsqueeze(2).to_broadcast([128, 8, 64]), op=ALU.mult),
                        reads=["qn", ("rs8", i % 2)], writes=["qn"])
                    cosb = cst[:, O_COSQ + i * 32:O_COSQ + (i + 1) * 32].unsqueeze(1).to_broadcast([128, 8, 32])
                    sinb = cst[:, O_SINQ + i * 32:O_SINQ + (i + 1) * 32].unsqueeze(1).to_broadcast([128, 8, 32])
                    r3 = [t.rearrange("p (h d) -> p h d", h=8) for t in rt]
                    qr3 = qrb.rearrange("p (h d) -> p h d", h=8)
                    x1, x2 = qn3[:, :, 0:32], qn3[:, :, 32:64]
                    S.add("dve", lambda e, x1=x1, cosb=cosb, r3=r3: e.tensor_tensor(out=r3[0], in0=x1, in1=cosb, op=ALU.mult),
                          reads=["qn", "cst"], writes=[("rt", 0)])
                    S.add("dve", lambda e, x2=x2, sinb=sinb, r3=r3: e.tensor_tensor(out=r3[1], in0=x2, in1=sinb, op=ALU.mult),
                          reads=["qn", "cst"], writes=[("rt", 1)])
                    S.add("dve", lambda e, r3=r3, qr3=qr3: e.tensor_tensor(out=qr3[:, :, 0:32], in0=r3[0], in1=r3[1], op=ALU.subtract),
                          reads=[("rt", 0), ("rt", 1)], writes=[("qr", i % 2)])
                    S.add("dve", lambda e, x2=x2, cosb=cosb, r3=r3: e.tensor_tensor(out=r3[2], in0=x2, in1=cosb, op=ALU.mult),
                          reads=["qn", "cst"], writes=[("rt", 2)])
                    S.add("dve", lambda e, x1=x1, sinb=sinb, r3=r3: e.tensor_tensor(out=r3[3], in0=x1, in1=sinb, op=ALU.mult),
                          reads=["qn", "cst"], writes=[("rt", 3)])
                    S.add("dve", lambda e, r3=r3, qr3=qr3: e.tensor_tensor(out=qr3[:, :, 32:64], in0=r3[2], in1=r3[3], op=ALU.add),
                          reads=[("rt", 2), ("rt", 3)], writes=[("qr", i % 2)])
                    pk = 6 + (i % 2)
                    for hp in range(4):
                        S.add("pe", lambda e, hp=hp, pk=pk, qrb=qrb: e.transpose(
                            out=psT_bf(pk)[:, hp * 128:(hp + 1) * 128], in_=qrb[:, hp * 128:(hp + 1) * 128],
                            identity=identb[:, 0:128]),
                            reads=[("qr", i % 2), "identb"], writes=[("ps", pk)])
                    S.add("act", lambda e, i=i, pk=pk, dstT=dstT: e.activation(
                        out=dstT[:, :, i * 128:(i + 1) * 128],
                        in_=psT_bf(pk)[:, 0:512].rearrange("p (h t) -> p h t", h=4), func=AF.Copy),
                        reads=[("ps", pk)], writes=[(gname + "T", i)])
                elif gname == "v":
                    S.add("act", lambda e, i=i, pp=pp: e.activation(
                        out=Vt[:, i, :, 0:64], in_=PS[pp][:].rearrange("p (h d) -> p h d", h=8), func=AF.Copy),
                        reads=[("ps", pp)], writes=[("Vt", i)])
                else:
                    S.add("act", lambda e, pp=pp: e.activation(out=iraw[:], in_=PS[pp][:, 0:324], func=AF.Copy),
                          reads=[("ps", pp)], writes=["iraw"])
                    qi3 = iraw[:, 0:256].rearrange("p (h d) -> p h d", h=4)
                    qib = qib_l[i % 2]
                    ki2b = ki2b_l[i % 2]
                    QIB = ("qib", i % 2)
                    KI2B = ("ki2b", i % 2)
                    qib3 = qib.rearrange("p (h d) -> p h d", h=4)
                    cosb = cst[:, O_COSI + i * 16:O_COSI + (i + 1) * 16].unsqueeze(1).to_broadcast([128, 4, 16])
                    sinb = cst[:, O_SINI + i * 16:O_SINI + (i + 1) * 16].unsqueeze(1).to_broadcast([128, 4, 16])
                    i3 = [t.rearrange("p (h d) -> p h d", h=4) for t in it]
                    x1, x2 = qi3[:, :, 0:16], qi3[:, :, 16:32]
                    S.add("dve", lambda e, x1=x1, cosb=cosb, i3=i3: e.tensor_tensor(out=i3[0], in0=x1, in1=cosb, op=ALU.mult),
                          reads=["iraw", "cst"], writes=[("it", 0)])
                    S.add("dve", lambda e, x2=x2, sinb=sinb, i3=i3: e.tensor_tensor(out=i3[1], in0=x2, in1=sinb, op=ALU.mult),
                          reads=["iraw", "cst"], writes=[("it", 1)])
                    S.add("dve", lambda e, i3=i3, qib3=qib3: e.tensor_tensor(out=qib3[:, :, 0:16], in0=i3[0], in1=i3[1], op=ALU.subtract),
                          reads=[("it", 0), ("it", 1)], writes=[QIB])
                    S.add("dve", lambda e, x2=x2, cosb=cosb, i3=i3: e.tensor_tensor(out=i3[2], in0=x2, in1=cosb, op=ALU.mult),
                          reads=["iraw", "cst"], writes=[("it", 2)])
                    S.add("dve", lambda e, x1=x1, sinb=sinb, i3=i3: e.tensor_tensor(out=i3[3], in0=x1, in1=sinb, op=ALU.mult),
                          reads=["iraw", "cst"], writes=[("it", 3)])
                    S.add("dve", lambda e, i3=i3, qib3=qib3: e.tensor_tensor(out=qib3[:, :, 16:32], in0=i3[2], in1=i3[3], op=ALU.add),
                          reads=[("it", 2), ("it", 3)], writes=[QIB])
                    S.add("dve", lambda e, qi3=qi3, qib3=qib3: e.tensor_copy(out=qib3[:, :, 32:64], in_=qi3[:, :, 32:64]),
                          reads=["iraw"], writes=[QIB])
                    kr = iraw[:, 256:320]
                    cos1 = cst[:, O_COSI + i * 16:O_COSI + (i + 1) * 16]
                    sin1 = cst[:, O_SINI + i * 16:O_SINI + (i + 1) * 16]
                    S.add("dve", lambda e, kr=kr, cos1=cos1: e.tensor_tensor(out=it[0][:, 0:16], in0=kr[:, 0:16], in1=cos1, op=ALU.mult),
                          reads=["iraw", "cst", QIB], writes=[("it", 0)])
                    S.add("dve", lambda e, kr=kr, sin1=sin1: e.tensor_tensor(out=it[1][:, 0:16], in0=kr[:, 16:32], in1=sin1, op=ALU.mult),
                          reads=["iraw", "cst", QIB], writes=[("it", 1)])
                    S.add("dve", lambda e, ki2b=ki2b: e.tensor_tensor(out=ki2b[:, 0:16], in0=it[0][:, 0:16], in1=it[1][:, 0:16], op=ALU.subtract),
                          reads=[("it", 0), ("it", 1)], writes=[KI2B])
                    S.add("dve", lambda e, kr=kr, cos1=cos1: e.tensor_tensor(out=it[2][:, 0:16], in0=kr[:, 16:32], in1=cos1, op=ALU.mult),
                          reads=["iraw", "cst", QIB], writes=[("it", 2)])
                    S.add("dve", lambda e, kr=kr, sin1=sin1: e.tensor_tensor(out=it[3][:, 0:16], in0=kr[:, 0:16], in1=sin1, op=ALU.mult),
                          reads=["iraw", "cst", QIB], writes=[("it", 3)])
                    S.add("dve", lambda e, ki2b=ki2b: e.tensor_tensor(out=ki2b[:, 16:32], in0=it[2][:, 0:16], in1=it[3][:, 0:16], op=ALU.add),
                          reads=[("it", 2), ("it", 3)], writes=[KI2B])
                    S.add("dve", lambda e, kr=kr, ki2b=ki2b: e.tensor_copy(out=ki2b[:, 32:64], in_=kr[:, 32:64]),
                          reads=["iraw"], writes=[KI2B])
                    S.add("dve", lambda e, ki2b=ki2b: e.tensor_copy(out=ki2b[:, 64:128], in_=ki2b[:, 0:64]),
                          reads=[KI2B], writes=[KI2B])
                    S.add("dve", lambda e, i=i: e.tensor_copy(out=wraw[:, i, :], in_=iraw[:, 320:324]),
                          reads=["iraw"], writes=[("wraw", i)])
                    pk = 6 + (i % 2)
                    for hp in range(2):
                        S.add("pe", lambda e, hp=hp, pk=pk, qib=qib: e.transpose(
                            out=psT_bf(pk)[:, hp * 128:(hp + 1) * 128], in_=qib[:, hp * 128:(hp + 1) * 128],
                            identity=identb[:, 0:128]),
                            reads=[QIB, "identb"], writes=[("ps", pk)])
                    S.add("pe", lambda e, pk=pk, ki2b=ki2b: e.transpose(
                        out=psT_bf(pk)[:, 256:384], in_=ki2b[:], identity=identb[:, 0:128]),
                        reads=[KI2B, "identb"], writes=[("ps", pk)])
                    S.add("act", lambda e, i=i, pk=pk: e.activation(
                        out=qiT[:, :, i * 128:(i + 1) * 128],
                        in_=psT_bf(pk)[:, 0:256].rearrange("p (h t) -> p h t", h=2), func=AF.Copy),
                        reads=[("ps", pk)], writes=[("qiT", i)])
                    S.add("act", lambda e, i=i, pk=pk: e.activation(
                        out=kiT2[:, i * 128:(i + 1) * 128], in_=psT_bf(pk)[:, 256:384], func=AF.Copy),
                        reads=[("ps", pk)], writes=[("kiT2", i)])
                tile_ops.append(S.end_capture())
            def _split(ops):
                for j, o in enumerate(ops):
                    if o[0] == 'pe' and any(w in (('ps', 6), ('ps', 7)) for w in o[3]):
                        return ops[:j], ops[j:]
                return ops, []
            parts = [_split(o) for o in tile_ops]
            for o in parts[0][0]:
                S.add(*o)
            for i in range(NT):
                if i + 1 < NT:
                    for o in parts[i + 1][0]:
                        S.add(*o)
                for o in parts[i][1]:
                    S.add(*o)
            if gname in ("v", "i"):
                grp_lists.append(S.end_capture())
        S.merge(grp_lists)
        wr2 = wraw[:].rearrange("p i h -> p (i h)")
        wa2 = wabs[:].rearrange("p i h -> p (i h)")
        ws2 = wsgn[:].rearrange("p i h -> p (i h)")
        allw = [("wraw", i) for i in range(NT)]
        S.add("dve", lambda e: e.tensor_scalar(out=wa2, in0=wr2, scalar1=-1.0, scalar2=None, op0=ALU.mult),
              reads=allw, writes=["wabs0"])
        S.add("dve", lambda e: e.tensor_tensor(out=wa2, in0=wa2, in1=wr2, op=ALU.max),
              reads=allw + ["wabs0"], writes=["wabs1"])
        S.add("dve", lambda e: e.tensor_scalar(out=wa2, in0=wa2, scalar1=1.0 / 16, scalar2=None, op0=ALU.mult),
              reads=["wabs1"], writes=["wabs"])
        S.add("dve", lambda e: e.tensor_scalar(out=ws2, in0=wr2, scalar1=0.0, scalar2=2.0, op0=ALU.is_ge, op1=ALU.mult),
              reads=allw, writes=["wsgn0"])
        S.add("dve", lambda e: e.tensor_scalar(out=ws2, in0=ws2, scalar1=-1.0, scalar2=None, op0=ALU.add),
              reads=["wsgn0"], writes=["wsgn"])

        dead = S.retire(lambda r: (isinstance(r, tuple) and r[0] in ("xnT", "xn_bf", "qraw", "rt", "qr", "it", "qib", "ki2b"))
                        or r in ("wst0", "wst1", "mixscr", "qsq", "qn", "iraw"))
        for nm in (["wouta", "attnT", "attn_tm"] + [("jm", a, b) for a in range(2) for b in range(2)]
                   + [("pT", i) for i in range(4)]
                   + [("acc", t, i) for t in range(2) for i in range(4)]):
            S.seed(nm, dead)
        QB = 256
        NQB = SEQ // QB
        accb = [view(RA, t * 8 * KB, 8 * KB, F32) for t in range(2)]
        junkD = view(RA, 16 * KB, 4 * KB, BF16)
        junkA = view(RA, 20 * KB, 4 * KB, BF16)
        jm4 = [view(RD, t * 4 * KB, 4 * KB, BF16) for t in range(4)]
        wouta = view(RD, 16 * KB, 8 * KB, BF16).rearrange("p (h d) -> p h d", h=4)
        attnT = view(RD, 24 * KB, 2 * KB, BF16).rearrange("p (h t) -> p h t", h=4)
        attn_tm = view(RD, 26 * KB, 2 * KB, BF16).rearrange("p (t c) -> p t c", t=2)
        pT = [view(RD, 28 * KB + i * KB, KB, BF16) for i in range(4)]
        S.add("pool", lambda e: e.dma_start(out=wouta[:], in_=wout_d[0:512, :].rearrange("(h p) d -> p h d", p=128)),
              writes=["wouta"], dma=True)
        trib = identb[:, 128:256]
        CAND, CNT, TT, MN, MX, TH = 0, 1, 2, 3, 4, 5
        pt_ctr = [0]

        def gen_A(qb):
            if qb == 0:
                j0, j1 = jm4[0], jm4[1]
                S.add("dve", lambda e: e.tensor_copy(out=j0[:, 0:128], in_=cbias), reads=["cst"], writes=[("jm", 0, 0)])
                S.add("dve", lambda e: e.memset(j1[:, 0:128], 0.0), writes=[("jm", 0, 1)])
                S.add("dve", lambda e: e.tensor_copy(out=j1[:, 128:256], in_=cbias), reads=["cst"], writes=[("jm", 0, 1)])
                return
            def tile_vars(tl):
                tq = 2 * qb + tl
                n = (tq + 1) * 128
                nchunk = (n + 511) // 512
                return tq, n, nchunk, accb[tl], jm4[(qb % 2) * 2 + tl], bis[:, tl * 8:(tl + 1) * 8], "b%d" % tl

            def stage_scores(tl):
                tq, n, nchunk, acc, jm, bb, bn = tile_vars(tl)
                for h in range(4):
                    hp, par = h // 2, h % 2
                    for sc in range(nchunk):
                        w = min(512, n - sc * 512)
                        pi = (h * nchunk + sc) % 2
                        S.add("pe", lambda e, hp=hp, par=par, sc=sc, w=w, pi=pi, tq=tq: e.matmul(
                            PS[pi][:, 0:w], lhsT=qiT[par * 64:(par + 1) * 64, hp, tq * 128:(tq + 1) * 128],
                            rhs=kiT2[par * 64:(par + 1) * 64, sc * 512:sc * 512 + w], start=True, stop=True),
                            reads=[("qiT", tq)] + [("kiT2", t) for t in range(sc * 4, sc * 4 + (w + 127) // 128)],
                            writes=[("ps", pi)])
                        S.add("act", lambda e, w=w, pi=pi, tq=tq, h=h: e.activation(
                            out=PS[pi][:, 0:w], in_=PS[pi][:, 0:w], func=AF.Relu, scale=wabs[:, tq, h:h + 1]),
                            reads=[("ps", pi), "wabs"], writes=[("ps", pi)])
                        if h == 0:
                            S.add("dve", lambda e, sc=sc, w=w, tq=tq, pi=pi, acc=acc: e.tensor_scalar(
                                out=acc[:, sc * 512:sc * 512 + w], in0=PS[pi][:, 0:w],
                                scalar1=wsgn[:, tq, 0:1], scalar2=None, op0=ALU.mult),
                                reads=[("ps", pi), "wsgn"], writes=[("acc", tl, sc)])
                        else:
                            S.add("dve", lambda e, sc=sc, w=w, tq=tq, h=h, pi=pi, acc=acc: e.scalar_tensor_tensor(
                                out=acc[:, sc * 512:sc * 512 + w], in0=PS[pi][:, 0:w],
                                scalar=wsgn[:, tq, h:h + 1], in1=acc[:, sc * 512:sc * 512 + w],
                                op0=ALU.mult, op1=ALU.add),
                                reads=[("ps", pi), "wsgn", ("acc", tl, sc)], writes=[("acc", tl, sc)])

            def stage_setup(tl):
                tq, n, nchunk, acc, jm, bb, bn = tile_vars(tl)
                accs = [("acc", tl, sc) for sc in range(nchunk)]
                S.add("dve", lambda e, n=n, acc=acc, bb=bb: e.tensor_reduce(out=bb[:, MN:MN + 1], in_=acc[:, 0:n], axis=AX.X, op=ALU.min),
                      reads=accs, writes=[bn + "mn"])
                S.add("dve", lambda e, tq=tq, acc=acc: e.tensor_tensor(out=acc[:, tq * 128:(tq + 1) * 128],
                                                                      in0=acc[:, tq * 128:(tq + 1) * 128], in1=cbias, op=ALU.add),
                      reads=accs + ["cst", bn + "mn"], writes=accs)
                S.add("dve", lambda e, n=n, acc=acc, bb=bb: e.tensor_reduce(out=bb[:, MX:MX + 1], in_=acc[:, 0:n], axis=AX.X, op=ALU.max),
                      reads=accs, writes=[bn + "mx"])
                S.add("dve", lambda e, bb=bb: e.tensor_tensor(out=bb[:, MX:MX + 1], in0=bb[:, MX:MX + 1], in1=bb[:, MN:MN + 1],
                                                              op=ALU.subtract), reads=[bn + "mx", bn + "mn"], writes=[bn + "rng"])
                S.add("dve", lambda e, bb=bb: e.tensor_scalar(out=bb[:, MX:MX + 1], in0=bb[:, MX:MX + 1], scalar1=1e-6,
                                                              scalar2=None, op0=ALU.max), reads=[bn + "rng"], writes=[bn + "rng2"])
                S.add("dve", lambda e, bb=bb: e.reciprocal(out=bb[:, MX:MX + 1], in_=bb[:, MX:MX + 1]),
                      reads=[bn + "rng2"], writes=[bn + "irng"])
                S.add("dve", lambda e, n=n, acc=acc, bb=bb: e.tensor_scalar(
                    out=acc[:, 0:n], in0=acc[:, 0:n], scalar1=bb[:, MN:MN + 1], scalar2=bb[:, MX:MX + 1],
                    op0=ALU.subtract, op1=ALU.mult),
                    reads=accs + [bn + "mn", bn + "irng"], writes=accs)
                S.add("dve", lambda e, bb=bb, tl=tl: e.memset(bb[:, CAND:CAND + 1], 0.5 if tl == 1 else -0.5),
                      writes=[bn + "cand"])

            def stage_iter(tl, it_):
                tq, n, nchunk, acc, jm, bb, bn = tile_vars(tl)
                accs = [("acc", tl, sc) for sc in range(nchunk)]
                if tl == 1:
                    S.add("dve", lambda e, n=n, acc=acc, bb=bb: e.tensor_scalar(
                        out=junkD[:, 0:n], in0=acc[:, 0:n], scalar1=bb[:, CAND:CAND + 1], scalar2=None,
                        op0=ALU.is_ge, op1=ALU.add, accum_out=bb[:, CNT:CNT + 1]),
                        reads=accs + [bn + "cand"], writes=[bn + "cnt"])
                    S.add("dve", lambda e, it_=it_, bb=bb: e.tensor_scalar(
                        out=bb[:, TT:TT + 1], in0=bb[:, CNT:CNT + 1], scalar1=TOPK - 0.5, scalar2=2.0 ** (-it_),
                        op0=ALU.is_ge, op1=ALU.mult), reads=[bn + "cnt"], writes=[bn + "tt"])
                    S.add("dve", lambda e, it_=it_, bb=bb: e.scalar_tensor_tensor(
                        out=bb[:, CAND:CAND + 1], in0=bb[:, TT:TT + 1], scalar=-(2.0 ** (-(it_ + 1))),
                        in1=bb[:, CAND:CAND + 1], op0=ALU.add, op1=ALU.add),
                        reads=[bn + "tt", bn + "cand"], writes=[bn + "cand"])
                else:
                    S.add("act", lambda e, n=n, acc=acc, bb=bb: e.activation(
                        out=junkA[:, 0:n], in_=acc[:, 0:n], func=AF.Sign, bias=bb[:, CAND:CAND + 1], scale=1.0,
                        accum_out=bb[:, CNT:CNT + 1]),
                        reads=accs + [bn + "cand"], writes=[bn + "cnt"])
                    S.add("act", lambda e, n=n, bb=bb: e.activation(
                        out=bb[:, TT:TT + 1], in_=bb[:, CNT:CNT + 1], func=AF.Sign, bias=float(n - 2 * TOPK + 1), scale=1.0),
                        reads=[bn + "cnt"], writes=[bn + "tt"])
                    S.add("act", lambda e, it_=it_, bb=bb: e.activation(
                        out=bb[:, CAND:CAND + 1], in_=bb[:, TT:TT + 1], func=AF.Identity,
                        bias=bb[:, CAND:CAND + 1], scale=-(2.0 ** (-(it_ + 1)))),
                        reads=[bn + "tt", bn + "cand"], writes=[bn + "cand"])

            def stage_mask(tl):
                tq, n, nchunk, acc, jm, bb, bn = tile_vars(tl)
                accs = [("acc", tl, sc) for sc in range(nchunk)]
                if tl == 1:
                    S.add("dve", lambda e, bb=bb: e.tensor_scalar(out=bb[:, TH:TH + 1], in0=bb[:, CAND:CAND + 1],
                                                                  scalar1=-(2.0 ** (-(NBIS + 1))), scalar2=None, op0=ALU.add),
                          reads=[bn + "cand"], writes=[bn + "th"])
                else:
                    S.add("dve", lambda e, bb=bb: e.tensor_scalar(out=bb[:, TH:TH + 1], in0=bb[:, CAND:CAND + 1],
                                                                  scalar1=-1.0, scalar2=-(2.0 ** (-(NBIS + 1))),
                                                                  op0=ALU.mult, op1=ALU.add),
                          reads=[bn + "cand"], writes=[bn + "th"])
                S.add("dve", lambda e, n=n, acc=acc, bb=bb, jm=jm: e.tensor_scalar(
                    out=jm[:, 0:n], in0=acc[:, 0:n], scalar1=bb[:, TH:TH + 1], scalar2=bis[:, 23:24],
                    op0=ALU.is_lt, op1=ALU.mult),
                    reads=accs + [bn + "th", "negbig"], writes=[("jm", qb % 2, tl)])

            for tl in range(2):
                stage_scores(tl)
                stage_setup(tl)
            for it_ in range(1, NBIS + 1):
                stage_iter(0, it_)
                stage_iter(1, it_)
            for tl in range(2):
                stage_mask(tl)

        def gen_B(qb):
            npair = qb + 1
            units = [(h, p) for h in range(8) for p in range(npair)]

            def front(k):
                h, p = units[k]
                hp, par = h // 2, h % 2
                pS = 2 + k % 2
                pti = k % 4
                for j in range(2):
                    sbk = 2 * p + j
                    c0 = 128 if sbk == 2 * qb + 1 else 0
                    S.add("pe", lambda e, hp=hp, par=par, sbk=sbk, c0=c0, pS=pS, j=j: e.matmul(
                        PS[pS][:, j * QB + c0:(j + 1) * QB], lhsT=kT[par * 64:(par + 1) * 64, hp, sbk * 128:(sbk + 1) * 128],
                        rhs=qT[par * 64:(par + 1) * 64, hp, qb * QB + c0:(qb + 1) * QB], start=(j == 0), stop=False,
                        skip_group_check=True),
                        reads=[("kT", sbk)] + [("qT", 2 * qb + t) for t in range(2)], writes=[("ps", pS)])
                    for tl in range(c0 // 128, 2):
                        jm = jm4[(qb % 2) * 2 + tl]
                        S.add("pe", lambda e, sbk=sbk, tl=tl, pS=pS, jm=jm, j=j: e.matmul(
                            PS[pS][:, j * QB + tl * 128:j * QB + (tl + 1) * 128], lhsT=jm[:, sbk * 128:(sbk + 1) * 128],
                            rhs=identb[:, 0:128], start=False, stop=(j == 1 and tl == 1), skip_group_check=True),
                            reads=[("jm", qb % 2, tl), "identb"], writes=[("ps", pS)])
                S.add("act", lambda e, pS=pS, pti=pti: e.activation(
                    out=pT[pti][:], in_=PS[pS][:], func=AF.Exp, scale=0.125),
                    reads=[("ps", pS)], writes=[("pT", pti)])

            def back(k):
                h, p = units[k]
                po = 4 + h % 2
                pti = k % 4
                for j in range(2):
                    sbk = 2 * p + j
                    c0 = 128 if sbk == 2 * qb + 1 else 0
                    for tl in range(c0 // 128, 2):
                        first = (sbk == 0 and tl == 0)
                        last = (sbk == 2 * qb + tl)
                        S.add("pe", lambda e, sbk=sbk, h=h, tl=tl, po=po, pti=pti, first=first, last=last, j=j: e.matmul(
                            PS[po][:, tl * 65:(tl + 1) * 65], lhsT=pT[pti][:, j * QB + tl * 128:j * QB + (tl + 1) * 128],
                            rhs=Vt[:, sbk, h, :], start=first, stop=last, skip_group_check=True),
                            reads=[("Vt", sbk), "V", ("pT", pti)], writes=[("ps", po)])
                if p == npair - 1:
                    pv = PS[po][:, 0:130].rearrange("p (t c) -> p t c", c=65)
                    rd = bis[:, 16 + 2 * (h % 2):18 + 2 * (h % 2)]
                    S.add("dve", lambda e, pv=pv, rd=rd: e.reciprocal(out=rd, in_=pv[:, :, 64]),
                          reads=[("ps", po)], writes=[("rden", h % 2)])
                    S.add("dve", lambda e, h=h, pv=pv, rd=rd: e.tensor_tensor(
                        out=attn_tm[:, :, h * 64:(h + 1) * 64], in0=pv[:, :, 0:64],
                        in1=rd.unsqueeze(2).to_broadcast([128, 2, 64]), op=ALU.mult),
                        reads=[("ps", po), ("rden", h % 2)], writes=["attn_tm"])

            front(0)
            for k in range(len(units)):
                if k + 1 < len(units):
                    front(k + 1)
                back(k)
            for tl in range(2):
                i = 2 * qb + tl
                for hp in range(4):
                    S.add("pe", lambda e, tl=tl, hp=hp: e.transpose(
                        out=psT_bf(7)[:, hp * 128:(hp + 1) * 128], in_=attn_tm[:, tl, hp * 128:(hp + 1) * 128],
                        identity=identb[:, 0:128]),
                        reads=["attn_tm", "identb"], writes=[("ps", 7)])
                S.add("act", lambda e, tl=tl: e.activation(
                    out=attnT[:, :, tl * 128:(tl + 1) * 128],
                    in_=psT_bf(7)[:, 0:512].rearrange("p (h t) -> p h t", h=4), func=AF.Copy),
                    reads=[("ps", 7)], writes=["attnT"])
                for dh in range(2):
                    for hp in range(4):
                        S.add("pe", lambda e, tl=tl, dh=dh, hp=hp: e.matmul(
                            PS[7][:], lhsT=attnT[:, hp, tl * 128:(tl + 1) * 128],
                            rhs=wouta[:, hp, dh * 512:(dh + 1) * 512], start=(hp == 0), stop=(hp == 3)),
                            reads=["attnT", "wouta"], writes=[("ps", 7)])
                    S.add("dve", lambda e, i=i, dh=dh: e.tensor_tensor(
                        out=resid[:, i, dh * 512:(dh + 1) * 512], in0=PS[7][:], in1=resid[:, i, dh * 512:(dh + 1) * 512],
                        op=ALU.add),
                        reads=[("ps", 7), ("resid", i, dh)], writes=[("resid", i, dh)])

        gen_A(0)
        for qb in range(NQB):
            S.begin_capture()
            gen_B(qb)
            lb = S.end_capture()
            la = []
            if qb + 1 < NQB:
                S.begin_capture()
                gen_A(qb + 1)
                la = S.end_capture()
            S.merge([lb, la])

        dead = S.retire(lambda r: (isinstance(r, tuple) and r[0] in ("acc", "jm", "maskT", "pT", "qT", "kT", "Vt", "qiT", "kiT2"))
                        or r in ("attnT", "attn_tm", "wouta", "rden", "V"))
        for nm in ([("xnT", i) for i in range(NT)] + ["grep", ("xn_bf", 0), ("xn_bf", 1), ("wdb", 0), ("wdb", 1),
                   ("wgb", 0), ("wgb", 1), ("wub", 0), ("wub", 1), ("sgb", 0), ("sgb", 1)]
                   + [("hT", c, t) for c in range(6) for t in range(4)]):
            S.seed(nm, dead)
        ffn(1, 2, False, True)
        S.add("sp", lambda e: e.nop(), reads=[("out", i) for i in range(NT)])
        S.emit(st)
    return nc


def _consts():
    c = np.zeros((128, NCST), np.float32)
    p = np.arange(128)
    c[:, O_CB:O_CB + 128] = np.where(p[None, :] <= p[:, None], 0.0, NEG).astype(np.float32)
    pos = (np.arange(NT)[None, :] * 128 + p[:, None]).astype(np.float32)
    for half, oc, os_ in ((32, O_COSQ, O_SINQ), (16, O_COSI, O_SINI)):
        inv = (np.float32(10000.0) ** (-np.arange(half, dtype=np.float32) / np.float32(half))).astype(np.float32)
        ang = (pos[:, :, None] * inv[None, None, :]).astype(np.float32)
        c[:, oc:oc + NT * half] = np.cos(ang).astype(np.float32).reshape(128, -1)
        c[:, os_:os_ + NT * half] = np.sin(ang).astype(np.float32).reshape(128, -1)
    c[:, O_INVC:O_INVC + 16] = (1.0 / np.arange(1, 17, dtype=np.float32))[None, :]
    return c


_PROG = None


def kernel(x, ffn1_norm, ffn1_w_gate, ffn1_w_up, ffn1_w_down, mix_norm, w_in, q_norm, k_norm,
           pool_w, pool_scale, w_out, ffn2_norm, ffn2_w_gate, ffn2_w_up, ffn2_w_down):
    global _PROG
    f = lambda a: np.ascontiguousarray(np.asarray(a, dtype=np.float32))
    x = f(x)
    cst = _consts()
    cst[:, O_G1:O_G1 + 8] = f(ffn1_norm)[0].reshape(8, 128).T
    cst[:, O_GM:O_GM + 8] = f(mix_norm)[0].reshape(8, 128).T
    cst[:, O_G2:O_G2 + 8] = f(ffn2_norm)[0].reshape(8, 128).T
    cst[:, O_GQ:O_GQ + 64] = f(q_norm)[0][None, :]
    cst[:, O_GK:O_GK + 64] = f(k_norm)[0][None, :]
    cst[:, O_PS:O_PS + 4] = f(pool_scale)[0].reshape(4, 128).T
    shared = {
        "ffn1_w_gate": f(ffn1_w_gate)[0], "ffn2_w_gate": f(ffn2_w_gate)[0],
        "ffn1_w_up": f(ffn1_w_up)[0], "ffn2_w_up": f(ffn2_w_up)[0],
        "ffn1_w_down": f(ffn1_w_down)[0], "ffn2_w_down": f(ffn2_w_down)[0],
        "w_in": f(w_in)[0], "w_out": f(w_out)[0], "pool_w": f(pool_w)[0],
        "grep": np.ascontiguousarray(np.broadcast_to(
            np.stack([f(ffn1_norm)[0], f(mix_norm)[0], f(ffn2_norm)[0]])[:, None, :], (3, 128, DM))),
        "cst": cst, "ident": np.concatenate([np.eye(128, dtype=np.float32), np.triu(np.ones((128, 128), np.float32))], axis=1),
    }
    if _PROG is None:
        _PROG = build_program()
    n = x.shape[0]
    in_maps = [dict(shared, x=x[b]) for b in range(n)]
    res = run_bass_kernel_spmd(_PROG, in_maps, core_ids=list(range(n)))
    return np.stack([np.asarray(r["out"], dtype=np.float32) for r in res.results], axis=0)
```
